# Optimizing a Trainium2 kernel written in Bass

```python
import math
import jax, jax.numpy as jnp
from jax import lax
import numpy as np

D_MODEL = 1024
BATCH = 8
SEQ = 2048
DEPTH = 1

N_MOD = 6
NORM_EPS = 1e-6

A_GROUPS = ((128, 1), (512, 4), (2048, 16))
A_N_GROUPS = 3
A_HEADS_PER_GROUP = 4
A_HEAD_DIM = 128
A_WIDTH = A_N_GROUPS * A_HEADS_PER_GROUP * A_HEAD_DIM
A_MERGED_WIDTH = A_HEADS_PER_GROUP * A_HEAD_DIM

B_HEADS = 8
B_HEAD_K = 128
B_HEAD_V = 128
B_KEY_WIDTH = B_HEADS * B_HEAD_K
B_VAL_WIDTH = B_HEADS * B_HEAD_V
B_CONV = 4
B_CHUNK = 64

MOE_GROUPS = 4
MOE_EXPERTS_PER_GROUP = 4
MOE_N_EXPERTS = MOE_GROUPS * MOE_EXPERTS_PER_GROUP
MOE_TOP_K = 2
MOE_D_FF = 256

IN_SPLITS = (A_WIDTH, A_WIDTH, A_WIDTH, B_KEY_WIDTH, B_KEY_WIDTH, B_VAL_WIDTH, B_VAL_WIDTH, B_HEADS, B_HEADS, D_MODEL, D_MODEL)
D_IN = sum(IN_SPLITS)

kernel_name = 'hybrid_dilated_gdn_hmoe_block'


def rms_norm(x, w):
    xf = x.astype(jnp.float32)
    y = xf * lax.rsqrt(jnp.mean(xf * xf, axis=-1, keepdims=True) + NORM_EPS)
    return (y * w.astype(jnp.float32)).astype(x.dtype)


def l2_norm(x):
    xf = x.astype(jnp.float32)
    return xf * lax.rsqrt(jnp.sum(xf * xf, axis=-1, keepdims=True) + NORM_EPS)


def _dilated_window_group(q, k, v, window, dilation):
    B, S, H, dh = q.shape
    span = window // dilation
    L = S // dilation
    nb = -(-L // span)
    Lp = nb * span

    def to_sub(t):
        e = t.shape[-1]
        t = t.reshape(B, L, dilation, H, e).transpose(0, 2, 3, 1, 4)
        t = jnp.pad(t, ((0, 0), (0, 0), (0, 0), (0, Lp - L), (0, 0)))
        return t.reshape(B, dilation, H, nb, span, e)

    def with_prev(t):
        prev = jnp.pad(t[:, :, :, :-1], ((0, 0), (0, 0), (0, 0), (1, 0), (0, 0), (0, 0)))
        return jnp.concatenate([prev, t], axis=4)

    qb = to_sub(q).astype(jnp.float32)
    kw = with_prev(to_sub(k)).astype(jnp.float32)
    vw = with_prev(to_sub(v))
    s = jnp.einsum('bdhnqe,bdhnke->bdhnqk', qb, kw) * (dh ** -0.5)
    qi = jnp.arange(span)[:, None]
    kj = jnp.arange(2 * span)[None, :]
    dist = qi + span - kj
    band = (dist >= 0) & (dist <= span)
    exists = (jnp.arange(nb)[:, None, None] > 0) | (kj[None] >= span)
    mask = band[None] & exists
    s = jnp.where(mask, s, -jnp.inf)
    m = jnp.max(s, axis=-1, keepdims=True)
    p = jnp.exp(s - m)
    den = jnp.sum(p, axis=-1, keepdims=True)
    o = jnp.einsum('bdhnqk,bdhnke->bdhnqe', p, vw) / den
    lse = (m + jnp.log(den))[..., 0]
    o = o.reshape(B, dilation, H, Lp, dh)[:, :, :, :L].transpose(0, 3, 1, 2, 4).reshape(B, S, H, dh)
    lse = lse.reshape(B, dilation, H, Lp)[..., :L].transpose(0, 3, 1, 2).reshape(B, S, H)
    return o, lse


def _dilated_attention_branch(qa, ka, va, q_norm_w, k_norm_w):
    B, S, _ = qa.shape
    shp = (B, S, A_N_GROUPS, A_HEADS_PER_GROUP, A_HEAD_DIM)
    q = rms_norm(qa.reshape(shp), q_norm_w)
    k = rms_norm(ka.reshape(shp), k_norm_w)
    v = va.reshape(shp)
    outs, lses = [], []
    for g, (window, dilation) in enumerate(A_GROUPS):
        o_g, lse_g = _dilated_window_group(q[:, :, g], k[:, :, g], v[:, :, g], window, dilation)
        outs.append(o_g)
        lses.append(lse_g)
    o = jnp.stack(outs, axis=0)
    lse = jnp.stack(lses, axis=0)
    w = jax.nn.softmax(lse, axis=0)
    out = jnp.sum(w[..., None] * o, axis=0)
    return out.reshape(B, S, A_MERGED_WIDTH).astype(qa.dtype)


def _causal_depthwise_conv(x, w):
    K, C = w.shape
    return lax.conv_general_dilated(x, w[:, None, :].astype(x.dtype), window_strides=(1,),
                                    padding=((K - 1, 0),), dimension_numbers=('NWC', 'WIO', 'NWC'),
                                    feature_group_count=C)


def _chunk_gated_delta_rule(q, k, v, g, beta):
    f32 = jnp.float32
    B, S, H, dk = q.shape
    dv = v.shape[-1]
    C = B_CHUNK
    N = S // C

    def chunks4(t):
        return t.astype(f32).reshape(B, N, C, H, t.shape[-1]).transpose(1, 0, 3, 2, 4)

    def chunks3(t):
        return t.astype(f32).reshape(B, N, C, H).transpose(1, 0, 3, 2)

    q = chunks4(q) * (dk ** -0.5)
    k = chunks4(k)
    v = chunks4(v)
    bt = chunks3(beta)
    gc = jnp.cumsum(chunks3(g), axis=-1)
    causal = jnp.tril(jnp.ones((C, C), dtype=bool))
    strict = jnp.tril(jnp.ones((C, C), dtype=bool), -1)
    diff = gc[..., :, None] - gc[..., None, :]
    decay = jnp.where(causal, jnp.exp(jnp.where(causal, diff, 0.0)), 0.0)
    kbeta = k * bt[..., None]
    lower = jnp.where(strict, jnp.einsum('nbhik,nbhjk->nbhij', kbeta, k) * decay, 0.0)
    rhs = jnp.concatenate([v * bt[..., None], kbeta * jnp.exp(gc)[..., None]], axis=-1)
    sol = lax.linalg.triangular_solve(lower + jnp.eye(C, dtype=f32), rhs, left_side=True,
                                      lower=True, unit_diagonal=True)
    u, w = sol[..., :dv], sol[..., dv:]
    intra = jnp.where(causal, jnp.einsum('nbhik,nbhjk->nbhij', q, k) * decay, 0.0)
    g_last = gc[..., -1]
    k_to_end = k * jnp.exp(g_last[..., None] - gc)[..., None]
    q_decay = q * jnp.exp(gc)[..., None]

    def step(state, xs):
        u_n, w_n, qd_n, intra_n, kend_n, glast_n = xs
        v_new = u_n - jnp.einsum('bhck,bhkv->bhcv', w_n, state)
        o_n = jnp.einsum('bhck,bhkv->bhcv', qd_n, state) + jnp.einsum('bhij,bhjv->bhiv', intra_n, v_new)
        state = state * jnp.exp(glast_n)[..., None, None] + jnp.einsum('bhck,bhcv->bhkv', kend_n, v_new)
        return state, o_n

    state0 = jnp.zeros((B, H, dk, dv), f32)
    _, o = lax.scan(step, state0, (u, w, q_decay, intra, k_to_end, g_last))
    return o.transpose(1, 0, 3, 2, 4).reshape(B, S, H, dv)


def _gated_deltanet_branch(qb, kb, vb, zb, beta_raw, a_raw, conv_w, A_log, dt_bias, out_norm_w):
    B, S, _ = qb.shape
    qkv = jax.nn.silu(_causal_depthwise_conv(jnp.concatenate([qb, kb, vb], axis=-1), conv_w))
    q, k, v = jnp.split(qkv, [B_KEY_WIDTH, 2 * B_KEY_WIDTH], axis=-1)
    q = l2_norm(q.reshape(B, S, B_HEADS, B_HEAD_K))
    k = l2_norm(k.reshape(B, S, B_HEADS, B_HEAD_K))
    v = v.reshape(B, S, B_HEADS, B_HEAD_V)
    beta = jax.nn.sigmoid(beta_raw.astype(jnp.float32))
    g = -jnp.exp(A_log.astype(jnp.float32)) * jax.nn.softplus(a_raw.astype(jnp.float32) + dt_bias.astype(jnp.float32))
    o = _chunk_gated_delta_rule(q, k, v, g, beta)
    o = rms_norm(o, out_norm_w) * jax.nn.silu(zb.reshape(B, S, B_HEADS, B_HEAD_V).astype(jnp.float32))
    return o.reshape(B, S, B_VAL_WIDTH).astype(qb.dtype)


def _token_mixers(h, w_in, conv_w, a_q_norm_w, a_k_norm_w, b_A_log, b_dt_bias, b_out_norm_w,
                  w_branch_a, w_branch_b, w_o):
    proj = jnp.einsum('bsd,de->bse', h, w_in)
    offsets = [int(o) for o in np.cumsum(IN_SPLITS)[:-1]]
    qa, ka, va, qb, kb, vb, zb, beta_raw, a_raw, gate_a_raw, gate_b_raw = jnp.split(proj, offsets, axis=-1)
    ya = _dilated_attention_branch(qa, ka, va, a_q_norm_w, a_k_norm_w)
    yb = _gated_deltanet_branch(qb, kb, vb, zb, beta_raw, a_raw, conv_w, b_A_log, b_dt_bias, b_out_norm_w)
    merged = (jax.nn.sigmoid(gate_a_raw) * jnp.einsum('bse,ed->bsd', ya, w_branch_a)
              + jax.nn.sigmoid(gate_b_raw) * jnp.einsum('bse,ed->bsd', yb, w_branch_b))
    return jnp.einsum('bsd,de->bse', merged, w_o)


def _hierarchical_moe(h, w_router_group, b_router_group, w_router_expert, b_router_expert,
                      w_gate, w_up, w_down):
    f32 = jnp.float32
    B, S, D = h.shape
    ht = h.reshape(B * S, D)
    T = ht.shape[0]
    group_logits = jnp.dot(ht, w_router_group).astype(f32) + b_router_group.astype(f32)
    p_group = jax.nn.softmax(group_logits, axis=-1)
    p_top_group, g_idx = lax.top_k(p_group, 1)
    expert_logits = (jnp.dot(ht, w_router_expert).astype(f32).reshape(T, MOE_GROUPS, MOE_EXPERTS_PER_GROUP)
                     + b_router_expert.astype(f32))
    in_group = jnp.einsum('tg,tge->te', jax.nn.one_hot(g_idx[:, 0], MOE_GROUPS, dtype=f32), expert_logits)
    top_logits, e_idx = lax.top_k(in_group, MOE_TOP_K)
    weights = p_top_group * jax.nn.softmax(top_logits, axis=-1)
    expert_ids = g_idx * MOE_EXPERTS_PER_GROUP + e_idx
    combine = jnp.sum(jax.nn.one_hot(expert_ids, MOE_N_EXPERTS, dtype=f32) * weights[..., None], axis=1)

    def expert(acc, xs):
        wg, wu, wd, cw = xs
        hidden = jax.nn.silu(ht @ wg) * (ht @ wu)
        return acc + (hidden @ wd).astype(f32) * cw[:, None], None

    acc, _ = lax.scan(expert, jnp.zeros((T, D), f32), (w_gate, w_up, w_down, combine.T))
    return acc.reshape(B, S, D).astype(h.dtype)


def setup_inputs(seed: int = 0) -> dict:
    key = jax.random.key(seed)
    ks = jax.random.split(key, 24)
    f32 = jnp.float32
    L = DEPTH
    D = D_MODEL

    def normal(k, shape, scale):
        return jax.random.normal(k, shape, f32) * scale

    def gain(k, shape):
        return 1.0 + 0.02 * jax.random.normal(k, shape, f32)

    dt = jnp.exp(jax.random.uniform(ks[9], (L, B_HEADS), f32, math.log(1e-3), math.log(1e-1)))
    return {
        'x': normal(ks[0], (BATCH, SEQ, D), 1.0),
        'c': normal(ks[1], (BATCH, D), 1.0),
        'w_ada': normal(ks[2], (L, D, N_MOD * D), 0.5 * D ** -0.5),
        'b_ada': normal(ks[3], (L, N_MOD * D), 0.02),
        'norm1_w': gain(ks[4], (L, D)),
        'w_in': normal(ks[5], (L, D, D_IN), D ** -0.5),
        'conv_w': normal(ks[6], (L, B_CONV, 2 * B_KEY_WIDTH + B_VAL_WIDTH), B_CONV ** -0.5),
        'a_q_norm_w': gain(ks[7], (L, A_HEAD_DIM)),
        'a_k_norm_w': gain(ks[8], (L, A_HEAD_DIM)),
        'b_A_log': jnp.log(jax.random.uniform(ks[10], (L, B_HEADS), f32, 1.0, 16.0)),
        'b_dt_bias': dt + jnp.log(-jnp.expm1(-dt)),
        'b_out_norm_w': gain(ks[11], (L, B_HEAD_V)),
        'w_branch_a': normal(ks[12], (L, A_MERGED_WIDTH, D), A_MERGED_WIDTH ** -0.5),
        'w_branch_b': normal(ks[13], (L, B_VAL_WIDTH, D), B_VAL_WIDTH ** -0.5),
        'w_o': normal(ks[14], (L, D, D), D ** -0.5),
        'norm2_w': gain(ks[15], (L, D)),
        'w_router_group': normal(ks[16], (L, D, MOE_GROUPS), D ** -0.5),
        'b_router_group': normal(ks[17], (L, MOE_GROUPS), 0.01),
        'w_router_expert': normal(ks[18], (L, D, MOE_N_EXPERTS), D ** -0.5),
        'b_router_expert': normal(ks[19], (L, MOE_GROUPS, MOE_EXPERTS_PER_GROUP), 0.01),
        'w_exp_gate': normal(ks[20], (L, MOE_N_EXPERTS, D, MOE_D_FF), D ** -0.5),
        'w_exp_up': normal(ks[21], (L, MOE_N_EXPERTS, D, MOE_D_FF), D ** -0.5),
        'w_exp_down': normal(ks[22], (L, MOE_N_EXPERTS, MOE_D_FF, D), MOE_D_FF ** -0.5),
    }


def reference(x, c, w_ada, b_ada, norm1_w, w_in, conv_w, a_q_norm_w, a_k_norm_w, b_A_log, b_dt_bias,
              b_out_norm_w, w_branch_a, w_branch_b, w_o, norm2_w, w_router_group, b_router_group,
              w_router_expert, b_router_expert, w_exp_gate, w_exp_up, w_exp_down):
    for i in range(DEPTH):
        mod = (jnp.dot(jax.nn.silu(c), w_ada[i]) + b_ada[i])[:, None, :]
        shift1, scale1, gate1, shift2, scale2, gate2 = jnp.split(mod, N_MOD, axis=-1)
        h = rms_norm(x, norm1_w[i]) * (1 + scale1) + shift1
        x = x + gate1 * _token_mixers(h, w_in[i], conv_w[i], a_q_norm_w[i], a_k_norm_w[i], b_A_log[i],
                                      b_dt_bias[i], b_out_norm_w[i], w_branch_a[i], w_branch_b[i], w_o[i])
        h2 = rms_norm(x, norm2_w[i]) * (1 + scale2) + shift2
        x = x + gate2 * _hierarchical_moe(h2, w_router_group[i], b_router_group[i], w_router_expert[i],
                                          b_router_expert[i], w_exp_gate[i], w_exp_up[i], w_exp_down[i])
    return x
```

```python
import contextlib
import numpy as np
import concourse.bass as bass
import concourse.mybir as mybir
from concourse.bass_utils import run_bass_kernel_spmd

F32 = mybir.dt.float32
BF16 = mybir.dt.bfloat16
AF = mybir.ActivationFunctionType
ALU = mybir.AluOpType

D = 1024
SEQ = 2048
NT = 16
D_IN = 10768
EPS = 1e-6
OFF_QA, OFF_KA, OFF_VA = 0, 1536, 3072
OFF_QB, OFF_KB, OFF_VB, OFF_ZB = 4608, 5632, 6656, 7680
OFF_BA = 8704
OFF_GA, OFF_GB = 8720, 9744

ENG = ("pe", "act", "dve", "pool", "sp")
DMA_POOL = 16


class _Rec:
    def __init__(self):
        self.calls = []

    def __getattr__(self, name):
        def f(*a, **k):
            self.calls.append((name, a, k))
            return None
        return f


def _record_calls(fn):
    r = _Rec()
    fn(r)
    assert r.calls
    return r.calls


class Sched:
    def __init__(self, nc, stack):
        self.nc = nc
        self.ops = {e: [] for e in ENG}
        self.cnt = {e: 0 for e in ENG}
        self.sem = {e: stack.enter_context(nc.semaphore("s_" + e)) for e in ENG if e != "sp"}
        self.dsem = {}
        for q in ("sp", "pool", "act"):
            self.dsem[q] = [stack.enter_context(nc.semaphore(f"d_{q}{i}")) for i in range(DMA_POOL)]
        self.dcnt = {q: 0 for q in ("sp", "pool", "act")}
        self.waited = {e: {} for e in ENG}
        self.lastw = {}
        self.readers = {}
        self.latest = {}

    def _need(self, eng, tok, waits):
        if tok is None:
            return
        semkey, sem, val = tok
        if self.waited[eng].get(semkey, 0) >= val:
            return
        self.waited[eng][semkey] = val
        waits.append((sem, val))

    def _deps(self, eng, reads, writes):
        waits = []
        for k in reads:
            self._need(eng, self.lastw.get(k), waits)
        for k in writes:
            self._need(eng, self.lastw.get(k), waits)
            for t in self.readers.get(k, ()):
                self._need(eng, t, waits)
        return waits

    def _record(self, tok, reads, writes):
        self.latest[tok[0]] = tok
        for k in reads:
            self.readers.setdefault(k, []).append(tok)
        for k in writes:
            self.lastw[k] = tok
            self.readers[k] = []

    @staticmethod
    def _excl(reads, writes):
        ps = [k for k in reads if isinstance(k, tuple) and k[0] == "ps"]
        if ps:
            reads = [k for k in reads if k not in ps]
            writes = list(writes) + ps
        return list(dict.fromkeys(reads)), list(dict.fromkeys(writes))

    def op(self, eng, fn, reads=(), writes=()):
        reads, writes = self._excl(reads, writes)
        waits = self._deps(eng, reads, writes)
        self.cnt[eng] += 1
        tok = (eng, self.sem[eng], self.cnt[eng])
        self.ops[eng].append((_record_calls(fn), waits, (self.sem[eng], 1)))
        self._record(tok, reads, writes)
        return tok

    def dma(self, q, fn, reads=(), writes=()):
        waits = self._deps(q, reads, writes)
        n = self.dcnt[q]
        self.dcnt[q] += 1
        slot, rnd = n % DMA_POOL, n // DMA_POOL
        sem = self.dsem[q][slot]
        semkey = (q, slot)
        if rnd > 0:
            self._need(q, (semkey, sem, 16 * rnd), waits)
        tok = (semkey, sem, 16 * (rnd + 1))
        self.ops[q].append((_record_calls(fn), waits, (sem, 16)))
        self._record(tok, reads, writes)
        return tok

    def barrier(self):
        toks = list(self.latest.values())
        for e in ENG:
            waits = []
            for t in toks:
                self._need(e, t, waits)
            if waits:
                self.ops[e].append((None, waits, None))
        self.lastw.clear()
        self.readers.clear()

    def final_wait(self, eng, toks):
        waits = []
        for t in toks:
            self._need(eng, t, waits)
        self.ops[eng].append((None, waits, None))

    def emit(self):
        nc = self.nc
        m = {"pe": "tensor", "act": "scalar", "dve": "vector", "pool": "gpsimd", "sp": "sync"}
        with nc.Block() as block:
            for e in ENG:
                lst = self.ops[e]
                if not lst:
                    continue

                def body(engine, lst=lst):
                    for fn, waits, inc in lst:
                        for sem, val in waits:
                            engine.wait_ge(sem, val)
                        if fn is None:
                            continue
                        for name, a, k in fn:
                            ins = getattr(engine, name)(*a, **k)
                        ins.then_inc(inc[0], inc[1])

                getattr(block, m[e])(body)


def _consts():
    i = np.arange(128)
    same = (i[:, None] // 64) == (i[None, :] // 64)
    c = {}
    c["ident"] = np.eye(128, dtype=np.float32)
    c["ones"] = np.ones((128, 128), np.float32)
    c["mle2"] = (same & (i[:, None] <= i[None, :])).astype(np.float32)
    c["mgt2"] = (same & (i[:, None] > i[None, :])).astype(np.float32)
    c["mlt2"] = (same & (i[:, None] < i[None, :])).astype(np.float32)
    c["same2"] = same.astype(np.float32)
    c["half0"] = np.repeat((i < 64).astype(np.float32)[:, None], 128, 1)
    c["half1"] = np.repeat((i >= 64).astype(np.float32)[:, None], 128, 1)
    c["mprev"] = (i[:, None] >= i[None, :]).astype(np.float32)
    c["mcur"] = (i[:, None] <= i[None, :]).astype(np.float32)
    names = list(c.keys())
    arr = np.concatenate([c[n] for n in names], axis=1)
    return names, arr


CONST_NAMES, CONST_ARR = _consts()
NCONST = len(CONST_NAMES)


def build(stop="all", dbg=()):
    nc = bass.Bass("TRN2", target_bir_lowering=False)

    def din(name, shape, dt=F32):
        return nc.dram_tensor(name, list(shape), dt, kind="ExternalInput").ap()

    x_d = din("x", [SEQ, D])
    c_d = din("c", [128, 8])
    wada_d = din("w_ada", [D, 6 * D])
    bada_d = din("b_ada", [1, 6 * D])
    n1w_d = din("n1w", [128, 8])
    n2w_d = din("n2w", [128, 8])
    win_d = din("w_in", [D, D_IN])
    cst_d = din("cst", [128, NCONST * 128])
    convw_d = din("conv_w", [128, 24 * 4])
    alog_d = din("a_log", [1, 8])
    dtb_d = din("dt_bias", [1, 8])
    onw_d = din("onw", [128, 1])
    wa_d = din("w_a", [512, D])
    wb_d = din("w_b", [D, D])
    wo_d = din("w_o", [D, D])
    wr_d = din("w_r", [D, 20])
    br_d = din("b_r", [1, 20])
    wg_d = din("w_eg", [16, D, 256])
    wu_d = din("w_eu", [16, D, 256])
    wd_d = din("w_ed", [16, 256, D])
    qnw_d = din("qnw", [128, 1])
    knw_d = din("knw", [128, 1])
    out_d = nc.dram_tensor("out", [SEQ, D], F32, kind="ExternalOutput").ap()
    dbg_d = {}

    def dbg_out(name, shape):
        if name in dbg:
            dbg_d[name] = nc.dram_tensor("dbg_" + name, list(shape), F32, kind="ExternalOutput").ap()
            return dbg_d[name]
        return None

    out_toks = []
    with contextlib.ExitStack() as st:
        S = Sched(nc, st)

        def sb(stack, name, shape, dt):
            return stack.enter_context(nc.sbuf_tensor(name, list(shape), dt))

        psum = [st.enter_context(nc.psum_tensor(f"ps{i}", [128, 512], F32)) for i in range(8)]
        ps_rr = [0]

        def ps_next():
            i = ps_rr[0]
            ps_rr[0] = (i + 1) % 8
            return i

        cst = sb(st, "cst_sb", [128, NCONST, 128], F32)
        S.dma("sp", lambda e: e.dma_start(out=cst[:].rearrange("p n m -> p (n m)"), in_=cst_d[:, :]), writes=["cst"])

        def C(name):
            return cst[:, CONST_NAMES.index(name), :]

        ident_bf = sb(st, "ident_bf", [128, 128], BF16)
        ones_bf = sb(st, "ones_bf", [128, 128], BF16)
        S.op("dve", lambda e: e.tensor_copy(out=ident_bf[:], in_=C("ident")), reads=["cst"], writes=["ident_bf"])
        S.op("dve", lambda e: e.tensor_copy(out=ones_bf[:], in_=C("ones")), reads=["cst"], writes=["ones_bf"])

        modT = sb(st, "modT", [128, 6, 8], F32)
        A1 = sb(st, "A1", [128, 8], F32)
        A2 = sb(st, "A2", [128, 8], F32)
        g1bc = sb(st, "g1bc", [128, D], F32)
        g2bc = sb(st, "g2bc", [128, D], F32)
        arena = sb(st, "arena", [128, 2 * 8 * SEQ], BF16)
        hT = arena[:, 0:8 * SEQ].rearrange("p (k t) -> p k t", k=8)
        yB = arena[:, 8 * SEQ:16 * SEQ].rearrange("p (k t) -> p k t", k=8)
        x1 = arena[:, :].bitcast(F32).rearrange("p (t f) -> p t f", t=NT)
        ba_tok = sb(st, "ba_tok", [128, NT, 16], F32)
        n2ss = sb(st, "n2ss", [128, NT], F32)

        def norm_stats(tag, src_tile, ss, junk):
            for t in range(NT):
                src, skeys = src_tile(t)
                S.op("act", lambda e, src=src, t=t: e.activation(out=junk[:], in_=src, func=AF.Square, accum_out=ss[:, t:t + 1]),
                     reads=skeys, writes=[tag + "junk", (tag + "ss", t)])

        p01 = contextlib.ExitStack()
        xt = [sb(p01, f"xt{i}", [128, D], F32) for i in range(2)]
        n1junk = sb(p01, "n1junk", [128, D], BF16)
        n1ss = sb(p01, "n1ss", [128, NT], F32)

        def src1(t):
            b = t % 2
            S.dma("sp", lambda e, t=t, b=b: e.dma_start(out=xt[b][:], in_=x_d[t * 128:(t + 1) * 128, :]),
                  writes=[("xt", b)])
            return xt[b][:], [("xt", b)]

        with contextlib.ExitStack() as ph:
            modbc = sb(ph, "modbc", [128, 6 * D], F32)
            csb = sb(ph, "csb", [128, 8], F32)
            cs = sb(ph, "cs", [128, 8], BF16)
            crep = sb(ph, "crep", [128, 8, 128], BF16)
            nw = sb(ph, "nw", [128, 2, 8], F32)
            wab = [sb(ph, f"wab{i}", [128, 8, 512], BF16) for i in range(2)]
            S.dma("sp", lambda e: e.dma_start(out=csb[:], in_=c_d[:, :]), writes=["csb"])
            S.dma("sp", lambda e: e.dma_start(out=nw[:, 0, :], in_=n1w_d[:, :]), writes=["nw0"])
            S.dma("sp", lambda e: e.dma_start(out=nw[:, 1, :], in_=n2w_d[:, :]), writes=["nw1"])
            S.dma("sp", lambda e: e.dma_start(out=modbc[:], in_=bada_d[0:1, :].partition_broadcast(128)),
                  writes=["modbc"])
            S.op("act", lambda e: e.activation(out=cs[:], in_=csb[:], func=AF.Silu), reads=["csb"], writes=["cs"])
            S.op("dve", lambda e: e.tensor_copy(out=crep[:], in_=cs[:].unsqueeze(2).to_broadcast([128, 8, 128])),
                 reads=["cs"], writes=["crep"])
            norm_stats("n1", src1, n1ss, n1junk)
            wada_v = wada_d.rearrange("(k p) n -> p k n", p=128)
            for n in range(12):
                wb = wab[n % 2]
                S.dma("pool", lambda e, wb=wb, n=n: e.dma_start(out=wb[:], in_=wada_v[:, :, n * 512:(n + 1) * 512]),
                      writes=[("wab", n % 2)])
                pi = ps_next()

                def mm(e, wb=wb, pi=pi):
                    for k in range(8):
                        r = e.matmul(psum[pi][:, :], lhsT=crep[:, k, :], rhs=wb[:, k, :], start=(k == 0), stop=(k == 7))
                    return r
                S.op("pe", mm, reads=["crep", ("wab", n % 2)], writes=[("ps", pi)])
                S.op("dve", lambda e, pi=pi, n=n: e.tensor_tensor(
                    out=modbc[:, n * 512:(n + 1) * 512], in0=psum[pi][:, :], in1=modbc[:, n * 512:(n + 1) * 512],
                    op=ALU.add), reads=[("ps", pi), "modbc"], writes=["modbc"])
            for v in range(6):
                for half in range(2):
                    pi = ps_next()

                    def tr(e, v=v, half=half, pi=pi):
                        for j in range(4):
                            c0 = v * D + (half * 4 + j) * 128
                            r = e.transpose(psum[pi][:, j * 128:(j + 1) * 128], modbc[:, c0:c0 + 128], C("ident"))
                        return r
                    S.op("pe", tr, reads=["modbc", "cst"], writes=[("ps", pi)])
                    S.op("dve", lambda e, v=v, half=half, pi=pi: e.tensor_copy(
                        out=modT[:, v, half * 4:half * 4 + 4], in_=psum[pi][:, 0:512:128]),
                        reads=[("ps", pi)], writes=["modT"])
            S.op("dve", lambda e: e.scalar_tensor_tensor(out=A1[:], in0=modT[:, 1, :], scalar=1.0, in1=nw[:, 0, :],
                                                         op0=ALU.add, op1=ALU.mult), reads=["modT", "nw0"], writes=["A1"])
            S.op("dve", lambda e: e.scalar_tensor_tensor(out=A2[:], in0=modT[:, 4, :], scalar=1.0, in1=nw[:, 1, :],
                                                         op0=ALU.add, op1=ALU.mult), reads=["modT", "nw1"], writes=["A2"])
            S.op("pool", lambda e: e.tensor_copy(out=g1bc[:], in_=modbc[:, 2 * D:3 * D]), reads=["modbc"], writes=["g1bc"])
            S.op("pool", lambda e: e.tensor_copy(out=g2bc[:], in_=modbc[:, 5 * D:6 * D]), reads=["modbc"], writes=["g2bc"])
            S.barrier()

        if stop == "p0":
            return _finish(nc, S, sb, st, out_d, out_toks, dbg_d)

        def norm_phase(tag, src_tile, Aw, shift_vec, hT_out, wsm, nsm, sm_out, ss_pre=None):
            with contextlib.ExitStack() as ph:
                if ss_pre is None:
                    junk = sb(ph, tag + "junk", [128, D], BF16)
                    ss = sb(ph, tag + "ss", [128, NT], F32)
                else:
                    ss = ss_pre
                rstd = sb(ph, tag + "rstd", [128, NT], F32)
                xn = [sb(ph, f"{tag}xn{i}", [128, D], F32) for i in range(2)]
                hf = [sb(ph, f"{tag}hf{i}", [128, 8, 128], F32) for i in range(2)]
                tmp = [sb(ph, f"{tag}tmp{i}", [128, 8, 128], F32) for i in range(2)]
                smT = [sb(ph, f"{tag}smT{i}", [32, 128], F32) for i in range(2)]
                if ss_pre is None:
                    norm_stats(tag, src_tile, ss, junk)
                S.op("act", lambda e: e.activation(out=rstd[:], in_=ss[:], func=AF.Ln, scale=1.0 / D, bias=EPS),
                     reads=[(tag + "ss", t) for t in range(NT)], writes=[tag + "rstd"])
                S.op("act", lambda e: e.activation(out=rstd[:], in_=rstd[:], func=AF.Exp, scale=-0.5),
                     reads=[tag + "rstd"], writes=[tag + "rstd"])

                def stage1(t):
                    src, skeys = src_tile(t)
                    b = t % 2
                    S.op("dve", lambda e, src=src, t=t, b=b: e.tensor_scalar(
                        out=xn[b][:], in0=src, scalar1=rstd[:, t:t + 1], scalar2=None, op0=ALU.mult),
                        reads=list(skeys) + [tag + "rstd"], writes=[(tag + "xn", b)])
                    pis = []
                    for half in range(2):
                        pi = ps_next()
                        pis.append(pi)

                        def tr(e, half=half, pi=pi, b=b):
                            for j in range(4):
                                k = half * 4 + j
                                e.transpose(psum[pi][:, j * 128:(j + 1) * 128], xn[b][:, k * 128:(k + 1) * 128], C("ident"))
                        S.op("pe", tr, reads=[(tag + "xn", b), "cst"], writes=[("ps", pi)])
                    return pis

                def stage2(t, pis):
                    b = t % 2
                    for half in range(2):
                        pi = pis[half]
                        S.op("dve", lambda e, half=half, pi=pi, b=b: e.tensor_tensor(
                            out=tmp[b][:, half * 4:half * 4 + 4, :],
                            in0=psum[pi][:, :].rearrange("p (j m) -> p j m", j=4),
                            in1=Aw[:, half * 4:half * 4 + 4].unsqueeze(2).to_broadcast([128, 4, 128]), op=ALU.mult),
                            reads=[("ps", pi), tag + "A"], writes=[(tag + "tmp", b, half)])
                        S.op("pool", lambda e, half=half, b=b: e.tensor_tensor(
                            out=hf[b][:, half * 4:half * 4 + 4, :], in0=tmp[b][:, half * 4:half * 4 + 4, :],
                            in1=modT[:, shift_vec, half * 4:half * 4 + 4].unsqueeze(2).to_broadcast([128, 4, 128]),
                            op=ALU.add), reads=[(tag + "tmp", b, half), "modT"], writes=[(tag + "hf", b, half)])
                    S.op("act", lambda e, t=t, b=b: e.copy(out=hT_out[:, :, t * 128:(t + 1) * 128], in_=hf[b][:]),
                         reads=[(tag + "hf", b, 0), (tag + "hf", b, 1)], writes=[(tag + "hT", t)])
                    pi = ps_next()

                    def smm(e, pi=pi, b=b):
                        for k in range(8):
                            e.matmul(psum[pi][0:nsm, 0:128], lhsT=wsm[:, k, :], rhs=hf[b][:, k, :], start=(k == 0), stop=(k == 7))
                    S.op("pe", smm, reads=[(tag + "hf", b, 0), (tag + "hf", b, 1), tag + "wsm"], writes=[("ps", pi)])
                    return pi

                def stage3(t, pi):
                    b = t % 2
                    S.op("dve", lambda e, pi=pi, b=b: e.tensor_copy(out=smT[b][0:nsm, :], in_=psum[pi][0:nsm, 0:128]),
                         reads=[("ps", pi)], writes=[(tag + "smT", b)])
                    pj = ps_next()
                    S.op("pe", lambda e, pj=pj, b=b: e.transpose(psum[pj][:, 0:nsm], smT[b][0:nsm, :], C("ident")[0:nsm, 0:nsm]),
                         reads=[(tag + "smT", b), "cst"], writes=[("ps", pj)])
                    S.op("dve", lambda e, pj=pj, t=t: e.tensor_copy(out=sm_out[:, t, :], in_=psum[pj][:, 0:nsm]),
                         reads=[("ps", pj)], writes=[(tag + "sm", t)])
                p1 = {0: stage1(0)}
                p2 = {}
                for t in range(NT):
                    if t + 1 < NT:
                        p1[t + 1] = stage1(t + 1)
                    p2[t] = stage2(t, p1[t])
                    if t >= 1:
                        stage3(t - 1, p2[t - 1])
                stage3(NT - 1, p2[NT - 1])
                S.barrier()

        with contextlib.ExitStack() as ph:
            wba = sb(ph, "wba", [128, 8, 16], F32)
            S.dma("sp", lambda e: e.dma_start(out=wba[:], in_=win_d.rearrange("(k p) n -> p k n", p=128)[:, :, OFF_BA:OFF_BA + 16]),
                  writes=["n1wsm"])
            norm_phase("n1", src1, A1, 0, hT, wba, 16, ba_tok, ss_pre=n1ss)
        p01.close()

        d = dbg_out("hT", [128, 8 * SEQ])
        if d is not None:
            with contextlib.ExitStack() as ph:
                hcp = sb(ph, "hcp", [128, 8 * SEQ], F32)
                S.op("dve", lambda e: e.tensor_copy(out=hcp[:], in_=hT[:].rearrange("p k t -> p (k t)")), writes=["hcp"])
                out_toks.append(S.dma("sp", lambda e, d=d: e.dma_start(out=d[:, :], in_=hcp[:]), reads=["hcp"]))
                S.barrier()
        d = dbg_out("ba", [128, NT * 16])
        if d is not None:
            out_toks.append(S.dma("sp", lambda e, d=d: e.dma_start(out=d[:, :], in_=ba_tok[:].rearrange("p t n -> p (t n)")),
                                  reads=[]))

        if stop == "p1":
            return _finish(nc, S, sb, st, out_d, out_toks, dbg_d)

        def dump_fm(name, tens, nk):
            d = dbg_out(name, [128, nk * SEQ])
            if d is None:
                return
            with contextlib.ExitStack() as ph:
                cp = sb(ph, "cp_" + name, [128, nk * SEQ], F32)
                S.op("dve", lambda e: e.tensor_copy(out=cp[:], in_=tens[:].rearrange("p k t -> p (k t)")), writes=["cp"])
                out_toks.append(S.dma("sp", lambda e, d=d: e.dma_start(out=d[:, :], in_=cp[:]), reads=["cp"]))
                S.barrier()

        deltanet_phase(nc, S, sb, st, psum, ps_next, C, ident_bf, ones_bf, hT, ba_tok, yB,
                       win_d, convw_d, alog_d, dtb_d, onw_d, dbg_out, out_toks)
        dump_fm("od", yB, 8)
        zgate_phase(nc, S, sb, psum, ps_next, hT, yB, win_d)
        dump_fm("yb", yB, 8)
        if stop == "dn":
            return _finish(nc, S, sb, st, out_d, out_toks, dbg_d)
        with contextlib.ExitStack() as mx:
            yA = sb(mx, "yA", [128, 4, SEQ], BF16)
            attention_phase(nc, S, sb, psum, ps_next, C, cst, ident_bf, ones_bf, hT, yA, win_d, qnw_d, knw_d)
            dump_fm("ya", yA, 4)
            if stop == "att":
                return _finish(nc, S, sb, st, out_d, out_toks, dbg_d)
            merged = sb(mx, "merged", [128, 8, SEQ], BF16)
            merge_phase(nc, S, sb, psum, ps_next, hT, yA, yB, merged, win_d, wa_d, wb_d)
            dump_fm("merged", merged, 8)
            if stop == "mrg":
                return _finish(nc, S, sb, st, out_d, out_toks, dbg_d)
            wo_phase(nc, S, sb, psum, ps_next, merged, x1, g1bc, x_d, wo_d, n2ss)
            S.barrier()
        d = dbg_out("x1", [128, NT * D])
        if d is not None:
            out_toks.append(S.dma("sp", lambda e, d=d: e.dma_start(out=d[:, :], in_=x1[:].rearrange("p t f -> p (t f)")),
                                  reads=[]))
        if stop == "wo":
            return _finish(nc, S, sb, st, out_d, out_toks, dbg_d)
        with contextlib.ExitStack() as fs:
            h2T = sb(fs, "h2T", [128, 8, SEQ], BF16)
            rl = sb(fs, "rl", [128, NT, 20], F32)
            with contextlib.ExitStack() as ph:
                wr = sb(ph, "wr", [128, 8, 20], F32)
                S.dma("sp", lambda e: e.dma_start(out=wr[:], in_=wr_d.rearrange("(k p) n -> p k n", p=128)), writes=["n2wsm"])
                norm_phase("n2", lambda t: (x1[:, t, :], []), A2, 3, h2T, wr, 20, rl, ss_pre=n2ss)
            moe_phase(nc, S, sb, psum, ps_next, C, ident_bf, h2T, rl, x1, g2bc, br_d, wg_d, wu_d, wd_d, out_d, out_toks)
        S.final_wait("sp", out_toks)
        S.emit()
        return nc, list(dbg_d.keys())


def pk(pi, q0=0, q1=4):
    return [("ps", pi)]


def deltanet_phase(nc, S, sb, st, psum, ps_next, C, ident_bf, ones_bf, hT, ba_tok, yB,
                   win_d, convw_d, alog_d, dtb_d, onw_d, dbg_out, out_toks):
    winv = win_d.rearrange("(k p) n -> p k n", p=128)
    with contextlib.ExitStack() as ph:
        alog = sb(ph, "alog", [128, 8], F32)
        dtb = sb(ph, "dtb", [128, 8], F32)
        onw = sb(ph, "onw_sb", [128, 1], F32)
        convw = sb(ph, "convw", [128, 24, 4], F32)
        S.dma("sp", lambda e: e.dma_start(out=alog[:], in_=alog_d[0:1, :].partition_broadcast(128)), writes=["alog"])
        S.dma("sp", lambda e: e.dma_start(out=dtb[:], in_=dtb_d[0:1, :].partition_broadcast(128)), writes=["dtb"])
        S.dma("sp", lambda e: e.dma_start(out=onw[:], in_=onw_d[:, :]), writes=["onw"])
        S.dma("sp", lambda e: e.dma_start(out=convw[:].rearrange("p c j -> p (c j)"), in_=convw_d[:, :]), writes=["convw"])
        bt = sb(ph, "bt", [128, NT, 8], F32)
        nbt = sb(ph, "nbt", [128, NT, 8], F32)
        gtk = sb(ph, "gtk", [128, NT, 8], F32)
        tmpa = sb(ph, "tmpa", [128, NT, 8], F32)
        negA = sb(ph, "negA", [128, 8], F32)
        gc = sb(ph, "gc", [128, NT, 8], F32)
        negegc = sb(ph, "negegc", [128, NT, 8], F32)
        ksc = sb(ph, "ksc", [128, NT, 8], F32)
        eglb = sb(ph, "eglb", [128, 2, NT, 8], F32)
        S.op("act", lambda e: e.activation(out=bt[:], in_=ba_tok[:, :, 0:8], func=AF.Sigmoid), writes=["bt"])
        S.op("dve", lambda e: e.tensor_scalar(out=nbt[:], in0=bt[:], scalar1=-1.0, scalar2=None, op0=ALU.mult),
             reads=["bt"], writes=["nbt"])
        S.op("dve", lambda e: e.tensor_tensor(out=tmpa[:], in0=ba_tok[:, :, 8:16],
                                              in1=dtb[:].unsqueeze(1).to_broadcast([128, NT, 8]), op=ALU.add),
             reads=["dtb"], writes=["tmpa"])
        S.op("act", lambda e: e.activation(out=tmpa[:], in_=tmpa[:], func=AF.Exp), reads=["tmpa"], writes=["tmpa"])
        S.op("act", lambda e: e.activation(out=tmpa[:], in_=tmpa[:], func=AF.Ln, bias=1.0), reads=["tmpa"], writes=["tmpa"])
        S.op("act", lambda e: e.activation(out=negA[:], in_=alog[:], func=AF.Exp), reads=["alog"], writes=["negA"])
        S.op("dve", lambda e: e.tensor_scalar(out=negA[:], in0=negA[:], scalar1=-1.0, scalar2=None, op0=ALU.mult),
             reads=["negA"], writes=["negA"])
        S.op("dve", lambda e: e.tensor_tensor(out=gtk[:], in0=tmpa[:], in1=negA[:].unsqueeze(1).to_broadcast([128, NT, 8]),
                                              op=ALU.mult), reads=["tmpa", "negA"], writes=["gtk"])
        g2 = gtk[:].rearrange("p t h -> p (t h)")
        pi = ps_next()

        def mmg(e, pi=pi):
            e.matmul(psum[pi][:, 0:128], lhsT=C("mle2"), rhs=g2, start=True, stop=True)
            e.matmul(psum[pi][:, 128:256], lhsT=C("same2"), rhs=g2, start=True, stop=True)
            e.matmul(psum[pi][:, 256:384], lhsT=C("half0"), rhs=g2, start=True, stop=True)
            return e.matmul(psum[pi][:, 384:512], lhsT=C("half1"), rhs=g2, start=True, stop=True)
        S.op("pe", mmg, reads=["gtk", "cst"], writes=pk(pi))
        S.op("dve", lambda e, pi=pi: e.tensor_copy(out=gc[:].rearrange("p t h -> p (t h)"), in_=psum[pi][:, 0:128]),
             reads=pk(pi), writes=["gc"])
        S.op("act", lambda e, pi=pi: e.activation(out=negegc[:].rearrange("p t h -> p (t h)"), in_=psum[pi][:, 0:128],
                                                  func=AF.Exp), reads=pk(pi), writes=["negegc"])
        S.op("dve", lambda e: e.tensor_scalar(out=negegc[:], in0=negegc[:], scalar1=-1.0, scalar2=None, op0=ALU.mult),
             reads=["negegc"], writes=["negegc"])
        S.op("dve", lambda e, pi=pi: e.tensor_tensor(out=ksc[:].rearrange("p t h -> p (t h)"), in0=psum[pi][:, 128:256],
                                                     in1=gc[:].rearrange("p t h -> p (t h)"), op=ALU.subtract),
             reads=pk(pi) + ["gc"], writes=["ksc"])
        S.op("act", lambda e: e.activation(out=ksc[:], in_=ksc[:], func=AF.Exp), reads=["ksc"], writes=["ksc"])
        S.op("act", lambda e, pi=pi: e.activation(out=eglb[:].rearrange("p a t h -> p (a t h)"), in_=psum[pi][:, 256:512],
                                                  func=AF.Exp), reads=pk(pi), writes=["eglb"])

        W = sb(ph, "dnW", [128, 8, 1536], BF16)
        diag = sb(ph, "diag", [128, 12, 4, 128], BF16)
        raw = sb(ph, "raw", [128, 12, 515], BF16)
        cs = sb(ph, "cs_dn", [128, 12, 512], BF16)
        sq = [sb(ph, f"sq_dn{i}", [128, 512], BF16) for i in range(3)]
        lnv = [sb(ph, f"lnv_dn{i}", [128, 512], F32) for i in range(3)]
        qdT = sb(ph, "qdT", [128, 4, 512], BF16)
        ktok = sb(ph, "ktok", [128, 4, 4, 128], BF16)
        vtok = sb(ph, "vtok", [128, 4, 4, 128], BF16)
        Gm2 = [sb(ph, f"Gm2_{i}", [128, 128], F32) for i in range(4)]
        EE = [sb(ph, f"EE{i}", [128, 256], F32) for i in range(4)]
        E2m = [sb(ph, f"E2m{i}", [128, 128], F32) for i in range(4)]
        E2s = [sb(ph, f"E2s{i}", [128, 128], F32) for i in range(4)]
        LB = [sb(ph, f"LB{i}", [128, 384], BF16) for i in range(8)]
        TT = sb(ph, "TT", [128, 4, 4, 128], BF16)
        intraT = sb(ph, "intraT", [128, 4, 4, 128], BF16)
        Sst = sb(ph, "Sst", [128, 4, 128], F32)
        Sbf = sb(ph, "Sbf", [128, 4, 128], BF16)
        Zt = sb(ph, "Zt", [128, 4, 128], BF16)
        vnew = sb(ph, "vnew", [128, 4, 128], BF16)
        obuf = sb(ph, "obuf", [128, 4, 512], F32)
        osq = sb(ph, "osq", [128, 512], BF16)
        lb_rr = [0]

        for hg in range(2):
            for ty, off in enumerate((OFF_QB, OFF_KB, OFF_VB)):
                S.dma("pool", lambda e, ty=ty, off=off: e.dma_start(
                    out=W[:, :, ty * 512:(ty + 1) * 512], in_=winv[:, :, off + hg * 512: off + (hg + 1) * 512]),
                    writes=[("dnW", ty)])
            for ci in range(12):
                gch = (ci // 4) * 8 + hg * 4 + (ci % 4)
                for j in range(4):
                    S.op("dve", lambda e, ci=ci, j=j, gch=gch: e.tensor_scalar(
                        out=diag[:, ci, j, :], in0=ident_bf[:], scalar1=convw[:, gch, j:j + 1], scalar2=None,
                        op0=ALU.mult), reads=["convw", "ident_bf"], writes=[("diag", ci)])
            S.op("pool", lambda e: e.memset(raw[:, :, 0:3], 0.0), writes=[("raw", ci) for ci in range(12)])
            S.op("dve", lambda e: e.memset(Sst[:], 0.0), writes=[("S", h) for h in range(4)])
            S.op("dve", lambda e: e.memset(Sbf[:], 0.0), writes=[("Sbf", h) for h in range(4)])

            for sc in range(4):
                tok0 = sc * 512
                for ci in range(12):
                    ty, hl = ci // 4, ci % 4
                    pi = ps_next()

                    def proj(e, ci=ci, ty=ty, hl=hl, pi=pi):
                        for k in range(8):
                            e.matmul(psum[pi][:, :], lhsT=W[:, k, ty * 512 + hl * 128: ty * 512 + (hl + 1) * 128],
                                     rhs=hT[:, k, tok0:tok0 + 512], start=(k == 0), stop=(k == 7))
                    S.op("pe", proj, reads=[("dnW", ty)], writes=pk(pi))
                    S.op("dve", lambda e, ci=ci, pi=pi: e.tensor_copy(out=raw[:, ci, 3:515], in_=psum[pi][:, :]),
                         reads=pk(pi), writes=[("raw", ci)])
                for ci in range(12):
                    pj = ps_next()

                    def conv(e, ci=ci, pj=pj):
                        for j in range(4):
                            e.matmul(psum[pj][:, :], lhsT=diag[:, ci, j, :], rhs=raw[:, ci, j:j + 512],
                                     start=(j == 0), stop=(j == 3))
                    S.op("pe", conv, reads=[("raw", ci), ("diag", ci)], writes=pk(pj))
                    S.op("act", lambda e, ci=ci, pj=pj: e.activation(out=cs[:, ci, :], in_=psum[pj][:, :], func=AF.Silu),
                         reads=pk(pj), writes=[("cs", ci)])
                    S.op("pool", lambda e, ci=ci: e.tensor_copy(out=raw[:, ci, 0:3], in_=raw[:, ci, 512:515]),
                         reads=[("raw", ci)], writes=[("raw", ci)])
                def tok_major(hl, src_ci, scaled):
                    pi = ps_next()
                    pbf = psum[pi][:, :].bitcast(BF16)

                    def trk(e, src_ci=src_ci, pbf=pbf):
                        for tt in range(4):
                            e.transpose(pbf[:, tt * 128:(tt + 1) * 128], cs[:, src_ci, tt * 128:(tt + 1) * 128], ident_bf[:])
                    S.op("pe", trk, reads=[("cs", src_ci), "ident_bf"], writes=pk(pi))
                    if scaled:
                        h = hg * 4 + hl
                        for tt in range(4):
                            T = sc * 4 + tt
                            S.op("dve", lambda e, hl=hl, tt=tt, T=T, h=h, pbf=pbf: e.tensor_scalar(
                                out=ktok[:, hl, tt, :], in0=pbf[:, tt * 128:(tt + 1) * 128], scalar1=ksc[:, T, h:h + 1],
                                scalar2=None, op0=ALU.mult), reads=pk(pi) + ["ksc"], writes=[("ktok", hl)])
                    else:
                        S.op("act", lambda e, hl=hl, pbf=pbf: e.copy(out=vtok[:, hl, :, :].rearrange("p a b -> p (a b)"),
                                                                    in_=pbf[:, 0:512]), reads=pk(pi), writes=[("vtok", hl)])
                for hl in range(4):
                    tok_major(hl, 8 + hl, False)
                order = [4, 5, 6, 7, 0, 1, 2, 3]
                pqs = {}

                def n_stage1(i):
                    ci = order[i]
                    lb = i % 3
                    S.op("dve", lambda e, ci=ci, lb=lb: e.tensor_tensor(out=sq[lb][:], in0=cs[:, ci, :], in1=cs[:, ci, :], op=ALU.mult),
                         reads=[("cs", ci)], writes=[("sq", lb)])
                    pq = ps_next()
                    pqs[i] = pq
                    S.op("pe", lambda e, pq=pq, lb=lb: e.matmul(psum[pq][:, :], lhsT=ones_bf[:], rhs=sq[lb][:], start=True, stop=True),
                         reads=[("sq", lb), "ones_bf"], writes=pk(pq))

                def n_stage2(i):
                    ci = order[i]
                    ty = ci // 4
                    lb = i % 3
                    pq = pqs[i]
                    S.op("act", lambda e, pq=pq, lb=lb: e.activation(out=lnv[lb][:], in_=psum[pq][:, :], func=AF.Ln, bias=EPS),
                         reads=pk(pq), writes=[("lnv", lb)])
                    S.op("act", lambda e, lb=lb: e.activation(out=lnv[lb][:], in_=lnv[lb][:], func=AF.Exp, scale=-0.5),
                         reads=[("lnv", lb)], writes=[("lnv", lb)])
                    scl = 128.0 ** -0.5 if ty == 0 else 1.0
                    S.op("dve", lambda e, ci=ci, lb=lb, scl=scl: e.scalar_tensor_tensor(
                        out=cs[:, ci, :], in0=cs[:, ci, :], scalar=scl, in1=lnv[lb][:], op0=ALU.mult, op1=ALU.mult),
                        reads=[("cs", ci), ("lnv", lb)], writes=[("cs", ci)])
                    if ty == 1:
                        tok_major(ci - 4, ci, True)
                n_stage1(0)
                n_stage1(1)
                for i in range(8):
                    if i + 2 < 8:
                        n_stage1(i + 2)
                    n_stage2(i)
                b_rr = [0]

                def ps_b():
                    i = 4 + b_rr[0] % 4
                    b_rr[0] += 1
                    return i

                def gen_B(tt):
                    T = sc * 4 + tt
                    tk = slice(tt * 128, (tt + 1) * 128)
                    cur = {}
                    pxs = {}
                    for hl in range(4):
                        h = hg * 4 + hl
                        S.op("dve", lambda e, bb=hl, h=h: e.tensor_scalar(
                            out=Gm2[bb][:], in0=C("mle2"), scalar1=gtk[:, T, h:h + 1], scalar2=None, op0=ALU.mult),
                            reads=["gtk", "cst"], writes=[("Gm2", hl)])
                    for hl in range(4):
                        px = ps_b()
                        pxs[hl] = px

                        h = hg * 4 + hl
                        S.op("pe", lambda e, bb=hl, px=px: e.matmul(psum[px][:, 0:128], lhsT=C("ones"), rhs=Gm2[bb][:], start=True, stop=True),
                             reads=[("Gm2", hl), "cst"], writes=pk(px))
                        S.op("dve", lambda e, bb=hl, px=px, h=h: e.tensor_scalar(
                            out=EE[bb][:, 0:128], in0=psum[px][:, 0:128], scalar1=gc[:, T, h:h + 1], scalar2=0.0,
                            op0=ALU.subtract, op1=ALU.min), reads=pk(px) + ["gc"], writes=[("EE", hl, 0)])
                        S.op("act", lambda e, bb=hl, px=px: e.activation(out=EE[bb][:, 128:256], in_=psum[px][:, 0:128], func=AF.Exp),
                             reads=pk(px), writes=[("EE", hl, 1)])
                        S.op("act", lambda e, bb=hl: e.activation(out=EE[bb][:, 0:128], in_=EE[bb][:, 0:128], func=AF.Exp),
                             reads=[("EE", hl, 0)], writes=[("EE", hl, 0)])
                    yield
                    for hl in range(4):
                        S.op("pool", lambda e, bb=hl: e.tensor_tensor(out=E2m[bb][:], in0=EE[bb][:, 0:128], in1=C("mle2"), op=ALU.mult),
                             reads=[("EE", hl, 0), "cst"], writes=[("E2m", hl)])
                        S.op("dve", lambda e, bb=hl: e.tensor_tensor(out=E2s[bb][:], in0=EE[bb][:, 0:128], in1=C("mlt2"), op=ALU.mult),
                             reads=[("EE", hl, 0), "cst"], writes=[("E2s", hl)])
                    yield
                    for hl in range(4):
                        h = hg * 4 + hl
                        bb = hl
                        py = ps_b()

                        def mmk(e, hl=hl, py=py):
                            e.matmul(psum[py][:, 0:128], lhsT=cs[:, 4 + hl, tk], rhs=cs[:, 4 + hl, tk], start=True, stop=True)
                            e.matmul(psum[py][:, 128:256], lhsT=cs[:, 4 + hl, tk], rhs=cs[:, hl, tk], start=True, stop=True)
                        S.op("pe", mmk, reads=[("cs", 4 + hl), ("cs", hl)], writes=pk(py))
                        l0 = 2 * hl
                        cur[hl] = l0
                        S.op("dve", lambda e, l0=l0, py=py, bb=bb, h=h: e.scalar_tensor_tensor(
                            out=LB[l0][:, 0:128], in0=psum[py][:, 0:128], scalar=nbt[:, T, h:h + 1], in1=E2s[bb][:],
                            op0=ALU.mult, op1=ALU.mult), reads=pk(py) + [("E2s", bb), "nbt"], writes=[("LB", l0, 0)])
                        S.op("dve", lambda e, hl=hl, py=py, bb=bb: e.tensor_tensor(
                            out=intraT[:, hl, tt, :], in0=psum[py][:, 128:256], in1=E2m[bb][:], op=ALU.mult),
                            reads=pk(py) + [("E2m", bb)], writes=[("intraT", hl, tt)])
                        S.op("pool", lambda e, hl=hl, bb=bb: e.tensor_tensor(
                            out=qdT[:, hl, tk], in0=cs[:, hl, tk], in1=EE[bb][:, 128:256], op=ALU.mult),
                            reads=[("cs", hl), ("EE", bb, 1)], writes=[("qdT", hl, tt)])
                        if hl % 2 == 1:
                            yield
                    for hl in range(4):
                        l0 = cur[hl]
                        pz = ps_b()
                        pzb = psum[pz][:, :].bitcast(BF16)
                        S.op("pe", lambda e, l0=l0, pzb=pzb: e.transpose(pzb[:, 0:128], LB[l0][:, 0:128], ident_bf[:]),
                             reads=[("LB", l0, 0), "ident_bf"], writes=pk(pz))
                        S.op("act", lambda e, l0=l0, pzb=pzb: e.copy(out=LB[l0][:, 256:384], in_=pzb[:, 0:128]),
                             reads=pk(pz), writes=[("LB", l0, 2)])
                    yield
                    for lev in range(6):
                        last = lev == 5
                        for hl in range(4):
                            c_ = cur[hl]
                            pl = ps_b()
                            if not last:
                                nxt = 2 * hl + (1 - (c_ % 2))

                                def mml(e, c_=c_, pl=pl, lev=lev):
                                    if lev == 0:
                                        e.matmul(psum[pl][:, 0:128], lhsT=LB[c_][:, 256:384], rhs=LB[c_][:, 0:128], start=True, stop=False)
                                        e.matmul(psum[pl][:, 128:256], lhsT=LB[c_][:, 256:384], rhs=ident_bf[:], start=False, stop=False)
                                        e.matmul(psum[pl][:, 128:256], lhsT=ident_bf[:], rhs=ident_bf[:], start=False, stop=False)
                                    else:
                                        e.matmul(psum[pl][:, 0:256], lhsT=LB[c_][:, 256:384], rhs=LB[c_][:, 0:256], start=True, stop=False)
                                        e.matmul(psum[pl][:, 128:256], lhsT=ident_bf[:], rhs=LB[c_][:, 128:256], start=False, stop=False)
                                    e.matmul(psum[pl][:, 256:384], lhsT=LB[c_][:, 0:128], rhs=LB[c_][:, 256:384], start=False, stop=True)
                                rdl = [("LB", c_, 0), ("LB", c_, 2), "ident_bf"] + ([("LB", c_, 1)] if lev > 0 else [])
                                S.op("pe", mml, reads=rdl, writes=pk(pl))
                                wr = [("LB", nxt, 0), ("LB", nxt, 1), ("LB", nxt, 2)]
                                if (lev + hl) % 2 == 0:
                                    S.op("act", lambda e, nxt=nxt, pl=pl: e.copy(out=LB[nxt][:, :], in_=psum[pl][:, 0:384]),
                                         reads=pk(pl), writes=wr)
                                else:
                                    S.op("dve", lambda e, nxt=nxt, pl=pl: e.tensor_copy(out=LB[nxt][:, :], in_=psum[pl][:, 0:384]),
                                         reads=pk(pl), writes=wr)
                                cur[hl] = nxt
                            else:
                                def mml2(e, c_=c_, pl=pl):
                                    e.matmul(psum[pl][:, 0:128], lhsT=LB[c_][:, 256:384], rhs=LB[c_][:, 128:256], start=True, stop=False)
                                    e.matmul(psum[pl][:, 0:128], lhsT=ident_bf[:], rhs=LB[c_][:, 128:256], start=False, stop=True)
                                S.op("pe", mml2, reads=[("LB", c_, 1), ("LB", c_, 2), "ident_bf"], writes=pk(pl))
                                if hl % 2 == 0:
                                    S.op("act", lambda e, hl=hl, pl=pl: e.copy(out=TT[:, hl, tt, :], in_=psum[pl][:, 0:128]),
                                         reads=pk(pl), writes=[("TT", hl, tt)])
                                else:
                                    S.op("dve", lambda e, hl=hl, pl=pl: e.tensor_copy(out=TT[:, hl, tt, :], in_=psum[pl][:, 0:128]),
                                         reads=pk(pl), writes=[("TT", hl, tt)])
                            if hl % 2 == 1:
                                yield

                def gen_C(tt):
                    T = sc * 4 + tt
                    for half in range(2):
                        b0 = 64 * half
                        rows = slice(b0, b0 + 64)
                        ctok = slice(tt * 128 + b0, tt * 128 + b0 + 64)
                        pA = [0, 1]
                        pB = [2, 3]
                        for pr in range(2):
                            def mm1(e, pr=pr):
                                for hl in (2 * pr, 2 * pr + 1):
                                    c0 = (hl % 2) * 128
                                    e.matmul(psum[pA[pr]][rows, c0:c0 + 128], lhsT=cs[:, 4 + hl, ctok], rhs=Sbf[:, hl, :],
                                             start=True, stop=True)
                            S.op("pe", mm1, reads=[("cs", 4 + 2 * pr), ("cs", 5 + 2 * pr), ("Sbf", 2 * pr), ("Sbf", 2 * pr + 1)],
                                 writes=pk(pA[pr]))
                        yield
                        for pr in range(2):
                            for hl in (2 * pr, 2 * pr + 1):
                                h = hg * 4 + hl
                                c0 = (hl % 2) * 128
                                S.op("dve", lambda e, hl=hl, c0=c0, pr=pr, h=h: e.scalar_tensor_tensor(
                                    out=Zt[rows, hl, :], in0=psum[pA[pr]][rows, c0:c0 + 128], scalar=negegc[rows, T, h:h + 1],
                                    in1=vtok[rows, hl, tt, :], op0=ALU.mult, op1=ALU.add),
                                    reads=pk(pA[pr]) + ["negegc", ("vtok", hl)], writes=[("Zt", hl, half)])
                        yield
                        for pr in range(2):
                            def mm2(e, pr=pr):
                                for hl in (2 * pr, 2 * pr + 1):
                                    c0 = (hl % 2) * 128
                                    e.matmul(psum[pB[pr]][rows, c0:c0 + 128], lhsT=TT[rows, hl, tt, rows], rhs=Zt[rows, hl, :],
                                             start=True, stop=True)
                            S.op("pe", mm2, reads=[("TT", 2 * pr, tt), ("TT", 2 * pr + 1, tt), ("Zt", 2 * pr, half), ("Zt", 2 * pr + 1, half)],
                                 writes=pk(pB[pr]))
                        yield
                        for pr in range(2):
                            for hl in (2 * pr, 2 * pr + 1):
                                h = hg * 4 + hl
                                c0 = (hl % 2) * 128
                                S.op("act", lambda e, hl=hl, c0=c0, pr=pr, h=h: e.activation(
                                    out=vnew[rows, hl, :], in_=psum[pB[pr]][rows, c0:c0 + 128], func=AF.Copy, scale=bt[rows, T, h:h + 1]),
                                    reads=pk(pB[pr]) + ["bt"], writes=[("vnew", hl, half)])
                        yield
                        pO = pA
                        pS = pB
                        for pr in range(2):
                            def mm3(e, pr=pr):
                                for hl in (2 * pr, 2 * pr + 1):
                                    c0 = (hl % 2) * 128
                                    e.matmul(psum[pO[pr]][:, c0:c0 + 64], lhsT=Sbf[:, hl, :], rhs=qdT[:, hl, ctok], start=True, stop=False)
                                    e.matmul(psum[pO[pr]][:, c0:c0 + 64], lhsT=vnew[rows, hl, :], rhs=intraT[rows, hl, tt, rows],
                                             start=False, stop=True)
                                for hl in (2 * pr, 2 * pr + 1):
                                    c0 = (hl % 2) * 128
                                    e.matmul(psum[pS[pr]][:, c0:c0 + 128], lhsT=ktok[rows, hl, tt, :], rhs=vnew[rows, hl, :],
                                             start=True, stop=True)
                            rd = []
                            for hl in (2 * pr, 2 * pr + 1):
                                rd += [("Sbf", hl), ("qdT", hl, tt), ("vnew", hl, half), ("intraT", hl, tt), ("ktok", hl)]
                            S.op("pe", mm3, reads=rd, writes=pk(pO[pr]) + pk(pS[pr]))
                        yield
                        for pr in range(2):
                            for hl in (2 * pr, 2 * pr + 1):
                                h = hg * 4 + hl
                                c0 = (hl % 2) * 128
                                S.op("dve", lambda e, hl=hl, c0=c0, pr=pr, h=h: e.scalar_tensor_tensor(
                                    out=Sst[:, hl, :], in0=Sst[:, hl, :], scalar=eglb[:, half, T, h:h + 1], in1=psum[pS[pr]][:, c0:c0 + 128],
                                    op0=ALU.mult, op1=ALU.add), reads=[("S", hl), "eglb"] + pk(pS[pr]), writes=[("S", hl)])
                                S.op("act", lambda e, hl=hl: e.copy(out=Sbf[:, hl, :], in_=Sst[:, hl, :]),
                                     reads=[("S", hl)], writes=[("Sbf", hl)])
                        yield
                        for pr in range(2):
                            for hl in (2 * pr, 2 * pr + 1):
                                c0 = (hl % 2) * 128
                                S.op("dve", lambda e, hl=hl, c0=c0, pr=pr: e.tensor_copy(
                                    out=obuf[:, hl, ctok], in_=psum[pO[pr]][:, c0:c0 + 64]),
                                    reads=pk(pO[pr]), writes=[("obuf", hl)])
                        yield

                def run_interleaved(gens):
                    gens = [g for g in gens if g is not None]
                    while gens:
                        for g in list(gens):
                            try:
                                next(g)
                            except StopIteration:
                                gens.remove(g)
                run_interleaved([gen_B(0)])
                for tt in range(4):
                    run_interleaved([gen_C(tt), gen_B(tt + 1) if tt < 3 else None])
                for hl in range(4):
                    h = hg * 4 + hl
                    lb = hl % 2
                    S.op("act", lambda e, hl=hl: e.activation(out=osq[:], in_=obuf[:, hl, :], func=AF.Square),
                         reads=[("obuf", hl)], writes=["osq"])
                    pq = ps_next()
                    S.op("pe", lambda e, pq=pq: e.matmul(psum[pq][:, :], lhsT=ones_bf[:], rhs=osq[:], start=True, stop=True),
                         reads=["osq", "ones_bf"], writes=pk(pq))
                    S.op("act", lambda e, pq=pq, lb=lb: e.activation(out=lnv[lb][:], in_=psum[pq][:, :], func=AF.Ln,
                                                                    scale=1.0 / 128, bias=EPS),
                         reads=pk(pq), writes=[("lnv", lb)])
                    S.op("act", lambda e, lb=lb: e.activation(out=lnv[lb][:], in_=lnv[lb][:], func=AF.Exp, scale=-0.5),
                         reads=[("lnv", lb)], writes=[("lnv", lb)])
                    S.op("dve", lambda e, hl=hl, h=h, lb=lb: e.scalar_tensor_tensor(
                        out=yB[:, h, tok0:tok0 + 512], in0=obuf[:, hl, :], scalar=onw[:, 0:1], in1=lnv[lb][:],
                        op0=ALU.mult, op1=ALU.mult), reads=[("obuf", hl), ("lnv", lb), "onw"], writes=[("yB", h)])
        S.barrier()


def zgate_phase(nc, S, sb, psum, ps_next, hT, yB, win_d):
    winv = win_d.rearrange("(k p) n -> p k n", p=128)
    with contextlib.ExitStack() as ph:
        Wz = sb(ph, "Wz", [128, 8, 1024], BF16)
        szb = [sb(ph, f"szb{i}", [128, 512], F32) for i in range(2)]
        for i in range(2):
            S.dma("pool", lambda e, i=i: e.dma_start(out=Wz[:, :, i * 512:(i + 1) * 512],
                                                     in_=winv[:, :, OFF_ZB + i * 512:OFF_ZB + (i + 1) * 512]), writes=[("Wz", i)])
        for h in range(8):
            for T4 in range(4):
                pi = ps_next()
                b = (h * 4 + T4) % 2

                def mz(e, h=h, T4=T4, pi=pi):
                    for k in range(8):
                        e.matmul(psum[pi][:, :], lhsT=Wz[:, k, h * 128:(h + 1) * 128], rhs=hT[:, k, T4 * 512:(T4 + 1) * 512],
                                 start=(k == 0), stop=(k == 7))
                S.op("pe", mz, reads=[("Wz", h // 4)], writes=pk(pi))
                S.op("act", lambda e, pi=pi, b=b: e.activation(out=szb[b][:], in_=psum[pi][:, :], func=AF.Silu),
                     reads=pk(pi), writes=[("szb", b)])
                S.op("dve", lambda e, h=h, T4=T4, b=b: e.tensor_tensor(
                    out=yB[:, h, T4 * 512:(T4 + 1) * 512], in0=yB[:, h, T4 * 512:(T4 + 1) * 512], in1=szb[b][:], op=ALU.mult),
                    reads=[("szb", b), ("yB", h)], writes=[("yB", h)])
        S.barrier()


def attention_phase(nc, S, sb, psum, ps_next, C, cst, ident_bf, ones_bf, hT, yA, win_d, qnw_d, knw_d):
    winv = win_d.rearrange("(k p) n -> p k n", p=128)
    imask = CONST_NAMES.index("mprev")
    mask2 = cst[:, imask:imask + 2, :].rearrange("p a b -> p (a b)")
    with contextlib.ExitStack() as ph:
        qkw = sb(ph, "qkw", [128, 2], F32)
        S.dma("sp", lambda e: e.dma_start(out=qkw[:, 0:1], in_=qnw_d[:, :]), writes=["qkw0"])
        S.dma("sp", lambda e: e.dma_start(out=qkw[:, 1:2], in_=knw_d[:, :]), writes=["qkw1"])
        Wb = [[sb(ph, f"attW{i}_{t}", [128, 8, 256], BF16) for t in range(3)] for i in range(2)]
        qP = sb(ph, "qP", [128, 2, SEQ], BF16)
        kP = sb(ph, "kP", [128, 2, SEQ], BF16)
        vP = sb(ph, "vP", [128, 16, 256], BF16)
        accn = sb(ph, "accn", [128, 2, SEQ], F32)
        accd = sb(ph, "accd", [128, 2, SEQ], F32)
        sq = [sb(ph, f"sq_at{i}", [128, 512], BF16) for i in range(2)]
        maskneg = sb(ph, "maskneg", [128, 256], BF16)
        S.op("dve", lambda e: e.tensor_scalar(out=maskneg[:], in0=mask2, scalar1=30000.0, scalar2=-30000.0,
                                              op0=ALU.mult, op1=ALU.add), reads=["cst"], writes=["maskneg"])
        lnv = [sb(ph, f"lnv_at{i}", [128, 512], F32) for i in range(2)]
        Eb = [sb(ph, f"Eb{i}", [128, 256], BF16) for i in range(4)]
        it = 0
        eb_rr = 0
        for hp in range(2):
            for g, d in enumerate((1, 4, 16)):
                L = SEQ // d
                nb = L // 128
                wb = Wb[it % 2]
                it += 1
                for t, off in enumerate((OFF_QA, OFF_KA, OFF_VA)):
                    c0 = off + g * 512 + hp * 256
                    S.dma("pool", lambda e, t=t, c0=c0, wb=wb: e.dma_start(out=wb[t][:], in_=winv[:, :, c0:c0 + 256]),
                          writes=[("attW", id(wb), t)])
                tiles = [(t, dst, j, T4) for t, dst in ((0, qP), (1, kP)) for j in range(2) for T4 in range(4)]

                def issue_proj(idx):
                    t, dst, j, T4 = tiles[idx]
                    pi = ps_next()

                    def pq(e, t=t, j=j, T4=T4, pi=pi, wb=wb):
                        for k in range(8):
                            e.matmul(psum[pi][:, :], lhsT=wb[t][:, k, j * 128:(j + 1) * 128],
                                     rhs=hT[:, k, T4 * 512:(T4 + 1) * 512], start=(k == 0), stop=(k == 7))
                    S.op("pe", pq, reads=[("attW", id(wb), t)], writes=pk(pi))
                    return pi
                pnext = issue_proj(0)
                for idx in range(len(tiles)):
                    t, dst, j, T4 = tiles[idx]
                    pi = pnext
                    if idx + 1 < len(tiles):
                        pnext = issue_proj(idx + 1)
                    lb = idx % 2
                    S.op("act", lambda e, pi=pi, lb=lb: e.activation(out=sq[lb][:], in_=psum[pi][:, :], func=AF.Square),
                         reads=pk(pi), writes=[("sq", lb)])
                    p2 = ps_next()
                    S.op("pe", lambda e, p2=p2, lb=lb: e.matmul(psum[p2][:, :], lhsT=ones_bf[:], rhs=sq[lb][:], start=True, stop=True),
                         reads=[("sq", lb)], writes=pk(p2))
                    S.op("act", lambda e, p2=p2, lb=lb: e.activation(out=lnv[lb][:], in_=psum[p2][:, :], func=AF.Ln,
                                                                    scale=1.0 / 128, bias=EPS),
                         reads=pk(p2), writes=[("lnv", lb)])
                    S.op("act", lambda e, lb=lb: e.activation(out=lnv[lb][:], in_=lnv[lb][:], func=AF.Exp, scale=-0.5),
                         reads=[("lnv", lb)], writes=[("lnv", lb)])
                    m0 = T4 * 512 // d
                    ov = dst[:, j, :].rearrange("p (r m) -> p r m", r=d)[:, :, m0:m0 + 512 // d]
                    S.op("dve", lambda e, pi=pi, lb=lb, ov=ov, t=t: e.scalar_tensor_tensor(
                        out=ov, in0=psum[pi][:, :].rearrange("p (m r) -> p r m", r=d), scalar=qkw[:, t:t + 1],
                        in1=lnv[lb][:].rearrange("p (m r) -> p r m", r=d), op0=ALU.mult, op1=ALU.mult),
                        reads=pk(pi) + [("lnv", lb), "qkw0", "qkw1"], writes=[("qkP", t, j)])
                for b2 in range(8):
                    pi = ps_next()

                    def pv(e, b2=b2, pi=pi, wb=wb):
                        for bb in range(2):
                            blk = b2 * 2 + bb
                            r, n = blk // nb, blk % nb
                            s0 = r + d * 128 * n
                            for k in range(8):
                                e.matmul(psum[pi][:, bb * 256:(bb + 1) * 256], lhsT=hT[:, k, s0:s0 + 127 * d + 1:d],
                                         rhs=wb[2][:, k, :], start=(k == 0), stop=(k == 7))
                    S.op("pe", pv, reads=[("attW", id(wb), 2)], writes=pk(pi))
                    S.op("act", lambda e, b2=b2, pi=pi: e.copy(out=vP[:, b2 * 2:b2 * 2 + 2, :].rearrange("p a b -> p (a b)"),
                                                               in_=psum[pi][:, :]), reads=pk(pi), writes=[("vP", b2)])
                blocks = [(j, blk) for j in range(2) for blk in range(16)]

                def issue_sc(idx):
                    nonlocal eb_rr
                    j, blk = blocks[idx]
                    n = blk % nb
                    kbs = ([(blk - 1, 0)] if n > 0 else []) + [(blk, 1)]
                    c_lo = kbs[0][1] * 128
                    psc = ps_next()

                    def sc(e, j=j, blk=blk, kbs=kbs, psc=psc, c_lo=c_lo):
                        for i, (kb, part) in enumerate(kbs):
                            e.matmul(psum[psc][:, part * 128:(part + 1) * 128], lhsT=kP[:, j, kb * 128:(kb + 1) * 128],
                                     rhs=qP[:, j, blk * 128:(blk + 1) * 128], start=(i == 0), stop=False)
                        e.matmul(psum[psc][:, c_lo:256], lhsT=ident_bf[:], rhs=maskneg[:, c_lo:256], start=False, stop=True)
                    S.op("pe", sc, reads=[("qkP", 0, j), ("qkP", 1, j), "maskneg"], writes=pk(psc))
                    eb = eb_rr % 4
                    eb_rr += 1
                    return psc, eb, kbs, c_lo
                nxt = issue_sc(0)
                pn = pd = None
                for idx in range(len(blocks)):
                    j, blk = blocks[idx]
                    b4, qb = blk // 4, blk % 4
                    psc, eb, kbs, c_lo = nxt
                    if idx + 1 < len(blocks):
                        nxt = issue_sc(idx + 1)
                    S.op("act", lambda e, psc=psc, eb=eb, c_lo=c_lo: e.activation(
                        out=Eb[eb][:, c_lo:256], in_=psum[psc][:, c_lo:256], func=AF.Exp, scale=128.0 ** -0.5),
                        reads=pk(psc), writes=[("Eb", eb)])
                    if qb == 0:
                        pn, pd = ps_next(), ps_next()

                    def pvm(e, j=j, qb=qb, kbs=kbs, eb=eb, pn=pn, pd=pd):
                        for i, (kb, part) in enumerate(kbs):
                            e.matmul(psum[pn][:, qb * 128:(qb + 1) * 128], lhsT=vP[:, kb, j * 128:(j + 1) * 128],
                                     rhs=Eb[eb][:, part * 128:(part + 1) * 128], start=(i == 0), stop=(i == len(kbs) - 1))
                        for i, (kb, part) in enumerate(kbs):
                            e.matmul(psum[pd][:, qb * 128:(qb + 1) * 128], lhsT=ones_bf[:],
                                     rhs=Eb[eb][:, part * 128:(part + 1) * 128], start=(i == 0), stop=(i == len(kbs) - 1))
                    S.op("pe", pvm, reads=[("Eb", eb)] + [("vP", kb // 2) for kb, _ in kbs], writes=pk(pn) + pk(pd))
                    if qb == 3:
                        for acc, pb in ((accn, pn), (accd, pd)):
                            if d == 1:
                                av = acc[:, j, b4 * 512:(b4 + 1) * 512]
                                bv = psum[pb][:, :]
                            elif d == 4:
                                av = acc[:, j, b4:SEQ:4]
                                bv = psum[pb][:, :]
                            else:
                                av = acc[:, j, :].rearrange("p (i s) -> p s i", s=16)[:, b4 * 4:b4 * 4 + 4, :]
                                bv = psum[pb][:, :].rearrange("p (r i) -> p r i", r=4)
                            if g == 0:
                                S.op("dve", lambda e, av=av, bv=bv: e.tensor_copy(out=av, in_=bv),
                                     reads=pk(pb), writes=[("acc", id(acc), j)])
                            else:
                                S.op("dve", lambda e, av=av, bv=bv: e.tensor_tensor(out=av, in0=av, in1=bv, op=ALU.add),
                                     reads=pk(pb) + [("acc", id(acc), j)], writes=[("acc", id(acc), j)])
            for j in range(2):
                S.op("act", lambda e, j=j: e.activation(out=accd[:, j, :], in_=accd[:, j, :], func=AF.Ln),
                     reads=[("acc", id(accd), j)], writes=[("acc", id(accd), j)])
                S.op("act", lambda e, j=j: e.activation(out=accd[:, j, :], in_=accd[:, j, :], func=AF.Exp, scale=-1.0),
                     reads=[("acc", id(accd), j)], writes=[("acc", id(accd), j)])
                S.op("dve", lambda e, j=j, hp=hp: e.tensor_tensor(out=yA[:, 2 * hp + j, :], in0=accn[:, j, :], in1=accd[:, j, :],
                                                                  op=ALU.mult),
                     reads=[("acc", id(accd), j), ("acc", id(accn), j)], writes=[("yA", 2 * hp + j)])
        S.barrier()


def merge_phase(nc, S, sb, psum, ps_next, hT, yA, yB, merged, win_d, wa_d, wb_d):
    winv = win_d.rearrange("(k p) n -> p k n", p=128)
    with contextlib.ExitStack() as ph:
        Wga = sb(ph, "Wga", [128, 8, D], BF16)
        Wgb = sb(ph, "Wgb", [128, 8, D], BF16)
        WA = sb(ph, "WA", [128, 4, D], BF16)
        WB = sb(ph, "WB", [128, 8, D], BF16)
        for i in range(2):
            cs_ = slice(i * 512, (i + 1) * 512)
            S.dma("pool", lambda e, i=i, cs_=cs_: e.dma_start(out=WA[:, :, cs_], in_=wa_d.rearrange("(k p) n -> p k n", p=128)[:, :, cs_]),
                  writes=[("WA", i)])
            S.dma("pool", lambda e, i=i, cs_=cs_: e.dma_start(out=Wga[:, :, cs_], in_=winv[:, :, OFF_GA + i * 512:OFF_GA + (i + 1) * 512]),
                  writes=[("Wga", i)])
            S.dma("pool", lambda e, i=i, cs_=cs_: e.dma_start(out=WB[:, :, cs_], in_=wb_d.rearrange("(k p) n -> p k n", p=128)[:, :, cs_]),
                  writes=[("WB", i)])
            S.dma("pool", lambda e, i=i, cs_=cs_: e.dma_start(out=Wgb[:, :, cs_], in_=winv[:, :, OFF_GB + i * 512:OFF_GB + (i + 1) * 512]),
                  writes=[("Wgb", i)])
        sg = [sb(ph, f"sg{i}", [128, 512], F32) for i in range(2)]
        m1 = [sb(ph, f"m1_{i}", [128, 512], F32) for i in range(2)]
        for c in range(8):
            cc = slice(c * 128, (c + 1) * 128)
            wi = c // 4
            for T4 in range(4):
                tk = slice(T4 * 512, (T4 + 1) * 512)
                for br, (Wp, nk, src, Wg, key) in enumerate(((WA, 4, yA, Wga, "A"), (WB, 8, yB, Wgb, "B"))):
                    pp, pg = ps_next(), ps_next()

                    def mmp(e, Wp=Wp, nk=nk, src=src, pp=pp, cc=cc, tk=tk):
                        for k in range(nk):
                            e.matmul(psum[pp][:, :], lhsT=Wp[:, k, cc], rhs=src[:, k, tk], start=(k == 0), stop=(k == nk - 1))
                    S.op("pe", mmp, reads=[("W" + key, wi)], writes=pk(pp))

                    def mmg(e, Wg=Wg, pg=pg, cc=cc, tk=tk):
                        for k in range(8):
                            e.matmul(psum[pg][:, :], lhsT=Wg[:, k, cc], rhs=hT[:, k, tk], start=(k == 0), stop=(k == 7))
                    S.op("pe", mmg, reads=[("Wg" + key.lower(), wi)], writes=pk(pg))
                    S.op("act", lambda e, pg=pg, br=br: e.activation(out=sg[br][:], in_=psum[pg][:, :], func=AF.Sigmoid),
                         reads=pk(pg), writes=[("sg", br)])
                    S.op("dve", lambda e, pp=pp, br=br: e.tensor_tensor(out=m1[br][:], in0=psum[pp][:, :], in1=sg[br][:], op=ALU.mult),
                         reads=pk(pp) + [("sg", br)], writes=[("m1", br)])
                S.op("pool", lambda e, c=c, tk=tk: e.tensor_tensor(out=merged[:, c, tk], in0=m1[0][:], in1=m1[1][:], op=ALU.add),
                     reads=[("m1", 0), ("m1", 1)], writes=[("merged", c)])
        S.barrier()


def wo_phase(nc, S, sb, psum, ps_next, merged, x1, g1bc, x_d, wo_d, n2ss):
    with contextlib.ExitStack() as ph:
        Wo = sb(ph, "Wo", [128, 8, D], BF16)
        for i in range(2):
            S.dma("pool", lambda e, i=i: e.dma_start(out=Wo[:, :, i * 512:(i + 1) * 512],
                                                     in_=wo_d.rearrange("(k p) n -> p k n", p=128)[:, :, i * 512:(i + 1) * 512]),
                  writes=[("Wo", i)])
        xt = [sb(ph, f"xt_wo{i}", [128, D], F32) for i in range(2)]
        tmp = [sb(ph, f"tmp_wo{i}", [128, 512], F32) for i in range(2)]
        junk = sb(ph, "wojunk", [128, D], BF16)
        for t in range(NT):
            b = t % 2
            S.dma("sp", lambda e, t=t, b=b: e.dma_start(out=xt[b][:], in_=x_d[t * 128:(t + 1) * 128, :]), writes=[("xt", b)])
            for ch in range(2):
                cc = slice(ch * 512, (ch + 1) * 512)
                pi = ps_next()

                def mm(e, t=t, cc=cc, pi=pi):
                    for k in range(8):
                        e.matmul(psum[pi][:, :], lhsT=merged[:, k, t * 128:(t + 1) * 128], rhs=Wo[:, k, cc], start=(k == 0), stop=(k == 7))
                S.op("pe", mm, reads=[("Wo", ch)], writes=pk(pi))
                S.op("dve", lambda e, pi=pi, ch=ch, cc=cc: e.tensor_tensor(out=tmp[ch][:], in0=psum[pi][:, :], in1=g1bc[:, cc], op=ALU.mult),
                     reads=pk(pi), writes=[("tmp", ch)])
                S.op("pool", lambda e, t=t, b=b, ch=ch, cc=cc: e.tensor_tensor(out=x1[:, t, cc], in0=tmp[ch][:], in1=xt[b][:, cc], op=ALU.add),
                     reads=[("tmp", ch), ("xt", b)], writes=[("x1", t, ch)])
            S.op("act", lambda e, t=t: e.activation(out=junk[:], in_=x1[:, t, :], func=AF.Square, accum_out=n2ss[:, t:t + 1]),
                 reads=[("x1", t, 0), ("x1", t, 1)], writes=["wojunk", ("n2ss", t)])
        S.barrier()


def moe_phase(nc, S, sb, psum, ps_next, C, ident_bf, h2T, rl, x1, g2bc, br_d, wg_d, wu_d, wd_d, out_d, out_toks):
    BIG = 1.0e30
    with contextlib.ExitStack() as ph:
        cwT = sb(ph, "cwT", [16, 2, SEQ], BF16)
        sel = sb(ph, "sel", [16, 16, 128], BF16)
        S.op("dve", lambda e: e.tensor_copy(out=sel[:], in_=ident_bf[0:16, 0:16].unsqueeze(2).to_broadcast([16, 16, 128])),
             writes=["sel"])
        Wgu = [sb(ph, f"Wgu{i}", [128, 2, 8, 256], BF16) for i in range(2)]

        def load_gu(ex2):
            wb2 = Wgu[ex2 % 2]
            S.dma("pool", lambda e: e.dma_start(out=wb2[:, 0, :, :], in_=wg_d[ex2].rearrange("(k p) f -> p k f", p=128)),
                  writes=[("Wgu", ex2 % 2, 0)])
            S.dma("pool", lambda e: e.dma_start(out=wb2[:, 1, :, :], in_=wu_d[ex2].rearrange("(k p) f -> p k f", p=128)),
                  writes=[("Wgu", ex2 % 2, 1)])
        load_gu(0)
        with contextlib.ExitStack() as rs:
            brb = sb(rs, "brb", [128, 20], F32)
            S.dma("sp", lambda e: e.dma_start(out=brb[:], in_=br_d[0:1, :].partition_broadcast(128)), writes=["brb"])
            gmax = sb(rs, "gmax", [128, NT], F32)
            ohg = sb(rs, "ohg", [128, NT, 4], F32)
            eg = sb(rs, "eg", [128, NT, 4], F32)
            ptop = sb(rs, "ptop", [128, NT], F32)
            pen = sb(rs, "pen", [128, NT, 4], F32)
            elm = sb(rs, "elm", [128, NT, 16], F32)
            m1 = sb(rs, "m1r", [128, NT], F32)
            m2 = sb(rs, "m2r", [128, NT], F32)
            oh1 = sb(rs, "oh1", [128, NT, 16], F32)
            oh2 = sb(rs, "oh2", [128, NT, 16], F32)
            w1 = sb(rs, "w1", [128, NT], F32)
            w2 = sb(rs, "w2", [128, NT], F32)
            cw = sb(rs, "cw", [128, NT, 16], F32)
            cwTf = sb(rs, "cwTf", [16, SEQ], F32)
            cwTr = sb(rs, "cwTr", [16, SEQ], F32)
            K = "rt"
            def bc(ap, n):
                return ap.unsqueeze(2).to_broadcast([128, NT, n])
            S.op("dve", lambda e: e.tensor_tensor(out=rl[:], in0=rl[:], in1=brb[:].unsqueeze(1).to_broadcast([128, NT, 20]), op=ALU.add),
                 reads=["brb"], writes=[K])
            gl = rl[:, :, 0:4]
            el = rl[:, :, 4:20]
            S.op("dve", lambda e: e.tensor_reduce(out=gmax[:], in_=gl, axis=mybir.AxisListType.X, op=ALU.max), reads=[K], writes=[K])
            S.op("dve", lambda e: e.tensor_tensor(out=ohg[:], in0=gl, in1=bc(gmax[:], 4), op=ALU.is_equal), reads=[K], writes=[K])
            S.op("dve", lambda e: e.tensor_tensor(out=eg[:], in0=gl, in1=bc(gmax[:], 4), op=ALU.subtract), reads=[K], writes=[K])
            S.op("act", lambda e: e.activation(out=eg[:], in_=eg[:], func=AF.Exp), reads=[K], writes=[K])
            S.op("dve", lambda e: e.tensor_reduce(out=ptop[:], in_=eg[:], axis=mybir.AxisListType.X, op=ALU.add), reads=[K], writes=[K])
            S.op("dve", lambda e: e.reciprocal(out=ptop[:], in_=ptop[:]), reads=[K], writes=[K])
            S.op("dve", lambda e: e.tensor_scalar(out=pen[:], in0=ohg[:], scalar1=BIG, scalar2=-BIG, op0=ALU.mult, op1=ALU.add),
                 reads=[K], writes=[K])
            S.op("dve", lambda e: e.tensor_tensor(out=elm[:].rearrange("p t (g e) -> p t g e", g=4),
                                                  in0=el.rearrange("p t (g e) -> p t g e", g=4),
                                                  in1=pen[:].unsqueeze(3).to_broadcast([128, NT, 4, 4]), op=ALU.add),
                 reads=[K], writes=[K])
            S.op("dve", lambda e: e.tensor_reduce(out=m1[:], in_=elm[:], axis=mybir.AxisListType.X, op=ALU.max), reads=[K], writes=[K])
            S.op("dve", lambda e: e.tensor_tensor(out=oh1[:], in0=elm[:], in1=bc(m1[:], 16), op=ALU.is_equal), reads=[K], writes=[K])
            S.op("dve", lambda e: e.scalar_tensor_tensor(out=elm[:].rearrange("p t e -> p (t e)"), in0=oh1[:].rearrange("p t e -> p (t e)"),
                                                         scalar=-BIG, in1=elm[:].rearrange("p t e -> p (t e)"),
                                                         op0=ALU.mult, op1=ALU.add), reads=[K], writes=[K])
            S.op("dve", lambda e: e.tensor_reduce(out=m2[:], in_=elm[:], axis=mybir.AxisListType.X, op=ALU.max), reads=[K], writes=[K])
            S.op("dve", lambda e: e.tensor_tensor(out=oh2[:], in0=elm[:], in1=bc(m2[:], 16), op=ALU.is_equal), reads=[K], writes=[K])
            S.op("dve", lambda e: e.tensor_tensor(out=w1[:], in0=m1[:], in1=m2[:], op=ALU.subtract), reads=[K], writes=[K])
            S.op("act", lambda e: e.activation(out=w1[:], in_=w1[:], func=AF.Sigmoid), reads=[K], writes=[K])
            S.op("dve", lambda e: e.tensor_scalar(out=w2[:], in0=w1[:], scalar1=-1.0, scalar2=1.0, op0=ALU.mult, op1=ALU.add),
                 reads=[K], writes=[K])
            S.op("dve", lambda e: e.tensor_tensor(out=w1[:], in0=w1[:], in1=ptop[:], op=ALU.mult), reads=[K], writes=[K])
            S.op("dve", lambda e: e.tensor_tensor(out=w2[:], in0=w2[:], in1=ptop[:], op=ALU.mult), reads=[K], writes=[K])
            S.op("dve", lambda e: e.tensor_tensor(out=oh1[:], in0=oh1[:], in1=bc(w1[:], 16), op=ALU.mult), reads=[K], writes=[K])
            S.op("dve", lambda e: e.tensor_tensor(out=oh2[:], in0=oh2[:], in1=bc(w2[:], 16), op=ALU.mult), reads=[K], writes=[K])
            S.op("dve", lambda e: e.tensor_tensor(out=cw[:], in0=oh1[:], in1=oh2[:], op=ALU.add), reads=[K], writes=[K])
            for q in range(4):
                pi = ps_next()

                def trc(e, q=q, pi=pi):
                    for j in range(4):
                        t = q * 4 + j
                        e.transpose(psum[pi][0:16, j * 128:(j + 1) * 128], cw[:, t, :], C("ident"))
                S.op("pe", trc, reads=[K, "cst"], writes=pk(pi))
                S.op("dve", lambda e, q=q, pi=pi: e.tensor_copy(out=cwTf[:, q * 512:(q + 1) * 512], in_=psum[pi][0:16, :]),
                     reads=pk(pi), writes=[("cwTf", q)])
            rq = [("cwTf", q) for q in range(4)]
            S.op("dve", lambda e: e.tensor_copy(out=cwT[:, 0, :], in_=cwTf[:]), reads=rq, writes=["cwT0"])
            S.op("dve", lambda e: e.tensor_tensor(out=cwTr[:], in0=cwTf[:], in1=cwT[:, 0, :], op=ALU.subtract),
                 reads=rq + ["cwT0"], writes=["cwTr"])
            S.op("dve", lambda e: e.tensor_copy(out=cwT[:, 1, :], in_=cwTr[:]), reads=["cwTr"], writes=["cwT1"])
            S.barrier()

        hid = sb(ph, "hid", [128, 4, 2, SEQ], BF16)
        Wd2 = [sb(ph, "Wd0", [128, 4, 2, D], BF16)] * 2
        sgb = [sb(ph, f"sgm{i}", [128, 512], F32) for i in range(2)]
        t1 = [sb(ph, f"t1m{i}", [128, 512], F32) for i in range(2)]
        tmo = [sb(ph, f"tmo{i}", [128, 512], F32) for i in range(2)]
        it = 0
        for gi in range(4):
            Wd = Wd2[gi % 2]
            for el_ in range(4):
                ex = gi * 4 + el_
                wb = Wgu[ex % 2]
                if ex + 1 < 16:
                    load_gu(ex + 1)
                S.dma("pool", lambda e, ex=ex, el_=el_, Wd=Wd: e.dma_start(out=Wd[:, el_, :, :], in_=wd_d[ex].rearrange("(c p) n -> p c n", p=128)),
                      writes=[("Wd", id(Wd), el_)])
                for T4 in range(4):
                    tk = slice(T4 * 512, (T4 + 1) * 512)
                    pc = ps_next()

                    def mmc(e, ex=ex, tk=tk, pc=pc):
                        e.matmul(psum[pc][:, :], lhsT=sel[:, ex, :], rhs=cwT[:, 0, tk], start=True, stop=False)
                        e.matmul(psum[pc][:, :], lhsT=sel[:, ex, :], rhs=cwT[:, 1, tk], start=False, stop=True)
                    S.op("pe", mmc, reads=["sel", "cwT0", "cwT1"], writes=pk(pc))
                    for ffc in range(2):
                        fc = slice(ffc * 128, (ffc + 1) * 128)
                        pg, pu = ps_next(), ps_next()

                        def mmgu(e, wb=wb, fc=fc, tk=tk, pg=pg, pu=pu):
                            for k in range(8):
                                e.matmul(psum[pg][:, :], lhsT=wb[:, 0, k, fc], rhs=h2T[:, k, tk], start=(k == 0), stop=(k == 7))
                            for k in range(8):
                                e.matmul(psum[pu][:, :], lhsT=wb[:, 1, k, fc], rhs=h2T[:, k, tk], start=(k == 0), stop=(k == 7))
                        S.op("pe", mmgu, reads=[("Wgu", ex % 2, 0), ("Wgu", ex % 2, 1)], writes=pk(pg) + pk(pu))
                        b = it % 2
                        it += 1
                        S.op("act", lambda e, pg=pg, b=b: e.activation(out=sgb[b][:], in_=psum[pg][:, :], func=AF.Silu),
                             reads=pk(pg), writes=[("sgm", b)])
                        S.op("dve", lambda e, pu=pu, b=b: e.tensor_tensor(out=t1[b][:], in0=psum[pu][:, :], in1=sgb[b][:], op=ALU.mult),
                             reads=pk(pu) + [("sgm", b)], writes=[("t1m", b)])
                        S.op("dve", lambda e, pc=pc, b=b, el_=el_, ffc=ffc, tk=tk: e.tensor_tensor(
                            out=hid[:, el_, ffc, tk], in0=psum[pc][:, :], in1=t1[b][:], op=ALU.mult),
                            reads=pk(pc) + [("t1m", b)], writes=[("hid", el_)])
            for t in range(NT):
                for ch in range(2):
                    cc = slice(ch * 512, (ch + 1) * 512)
                    po = ps_next()

                    def mmd(e, t=t, cc=cc, po=po, Wd=Wd):
                        i = 0
                        for e2 in range(4):
                            for ffc in range(2):
                                e.matmul(psum[po][:, :], lhsT=hid[:, e2, ffc, t * 128:(t + 1) * 128], rhs=Wd[:, e2, ffc, cc],
                                         start=(i == 0), stop=(i == 7))
                                i += 1
                    S.op("pe", mmd, reads=[("hid", e2) for e2 in range(4)] + [("Wd", id(Wd), e2) for e2 in range(4)], writes=pk(po))
                    S.op("dve", lambda e, po=po, ch=ch, cc=cc: e.tensor_tensor(out=tmo[ch][:], in0=psum[po][:, :], in1=g2bc[:, cc], op=ALU.mult),
                         reads=pk(po), writes=[("tmo", ch)])
                    S.op("pool", lambda e, t=t, ch=ch, cc=cc: e.tensor_tensor(out=x1[:, t, cc], in0=x1[:, t, cc], in1=tmo[ch][:], op=ALU.add),
                         reads=[("tmo", ch), ("x1", t, ch)], writes=[("x1", t, ch)])
                if gi == 3:
                    out_toks.append(S.dma("sp", lambda e, t=t: e.dma_start(out=out_d[t * 128:(t + 1) * 128, :], in_=x1[:, t, :]),
                                          reads=[("x1", t, 0), ("x1", t, 1)]))
        S.barrier()


def _finish(nc, S, sb, st, out_d, out_toks, dbg_d, zero_out=True):
    if zero_out:
        with contextlib.ExitStack() as ph:
            z = sb(ph, "zz", [128, D], F32)
            S.op("dve", lambda e: e.memset(z[:], 0.0), writes=["zz"])
            for t in range(NT):
                out_toks.append(S.dma("sp", lambda e, t=t: e.dma_start(out=out_d[t * 128:(t + 1) * 128, :], in_=z[:]),
                                      reads=["zz"]))
            S.barrier()
    S.final_wait("sp", out_toks)
    S.emit()
    return nc, list(dbg_d.keys())


def _prep_inputs(inputs):
    f = lambda a: np.ascontiguousarray(np.asarray(a, dtype=np.float32))
    shared = {
        "w_ada": f(inputs["w_ada"][0]),
        "b_ada": f(inputs["b_ada"][0][None, :]),
        "n1w": f(inputs["norm1_w"][0].reshape(8, 128).T),
        "n2w": f(inputs["norm2_w"][0].reshape(8, 128).T),
        "w_in": f(inputs["w_in"][0]),
        "cst": f(CONST_ARR),
        "conv_w": f(inputs["conv_w"][0].reshape(4, 24, 128).transpose(2, 1, 0).reshape(128, 96)),
        "a_log": f(inputs["b_A_log"][0][None, :]),
        "dt_bias": f(inputs["b_dt_bias"][0][None, :]),
        "onw": f(inputs["b_out_norm_w"][0][:, None]),
        "qnw": f(inputs["a_q_norm_w"][0][:, None]),
        "w_a": f(inputs["w_branch_a"][0]),
        "w_b": f(inputs["w_branch_b"][0]),
        "w_o": f(inputs["w_o"][0]),
        "w_r": f(np.concatenate([inputs["w_router_group"][0], inputs["w_router_expert"][0]], axis=1)),
        "b_r": f(np.concatenate([inputs["b_router_group"][0].reshape(-1), inputs["b_router_expert"][0].reshape(-1)])[None, :]),
        "w_eg": f(inputs["w_exp_gate"][0]),
        "w_eu": f(inputs["w_exp_up"][0]),
        "w_ed": f(inputs["w_exp_down"][0]),
        "knw": f(inputs["a_k_norm_w"][0][:, None]),
    }
    maps = []
    for b in range(8):
        m = dict(shared)
        m["x"] = f(inputs["x"][b])
        m["c"] = f(inputs["c"][b].reshape(8, 128).T)
        maps.append(m)
    return maps


def kernel(**inputs):
    nc, _ = build()
    maps = _prep_inputs(inputs)
    res = run_bass_kernel_spmd(nc, maps, core_ids=list(range(8)))
    return np.stack([np.asarray(r["out"], dtype=np.float32) for r in res.results], axis=0)
```

```python
import contextlib
import numpy as np
import concourse.bass as bass
import concourse.mybir as mybir
from concourse.bass_utils import run_bass_kernel_spmd

F32 = mybir.dt.float32
BF16 = mybir.dt.bfloat16
AF = mybir.ActivationFunctionType
ALU = mybir.AluOpType

D = 1024
SEQ = 2048
NT = 16
D_IN = 10768
EPS = 1e-6
OFF_QA, OFF_KA, OFF_VA = 0, 1536, 3072
OFF_QB, OFF_KB, OFF_VB, OFF_ZB = 4608, 5632, 6656, 7680
OFF_BA = 8704
OFF_GA, OFF_GB = 8720, 9744

ENG = ("pe", "act", "dve", "pool", "sp")
DMA_POOL = 16


class _Rec:
    def __init__(self):
        self.calls = []

    def __getattr__(self, name):
        def f(*a, **k):
            self.calls.append((name, a, k))
            return None
        return f


def _record_calls(fn):
    r = _Rec()
    fn(r)
    assert r.calls
    return r.calls


class Sched:
    def __init__(self, nc, stack):
        self.nc = nc
        self.ops = {e: [] for e in ENG}
        self.cnt = {e: 0 for e in ENG}
        self.sem = {e: stack.enter_context(nc.semaphore("s_" + e)) for e in ENG if e != "sp"}
        self.dsem = {}
        for q in ("sp", "pool", "act"):
            self.dsem[q] = [stack.enter_context(nc.semaphore(f"d_{q}{i}")) for i in range(DMA_POOL)]
        self.dcnt = {q: 0 for q in ("sp", "pool", "act")}
        self.waited = {e: {} for e in ENG}
        self.lastw = {}
        self.readers = {}
        self.latest = {}

    def _need(self, eng, tok, waits):
        if tok is None:
            return
        semkey, sem, val = tok
        if self.waited[eng].get(semkey, 0) >= val:
            return
        self.waited[eng][semkey] = val
        waits.append((sem, val))

    def _deps(self, eng, reads, writes):
        waits = []
        for k in reads:
            self._need(eng, self.lastw.get(k), waits)
        for k in writes:
            self._need(eng, self.lastw.get(k), waits)
            for t in self.readers.get(k, ()):
                self._need(eng, t, waits)
        return waits

    def _record(self, tok, reads, writes):
        self.latest[tok[0]] = tok
        for k in reads:
            self.readers.setdefault(k, []).append(tok)
        for k in writes:
            self.lastw[k] = tok
            self.readers[k] = []

    @staticmethod
    def _excl(reads, writes):
        ps = [k for k in reads if isinstance(k, tuple) and k[0] == "ps"]
        if ps:
            reads = [k for k in reads if k not in ps]
            writes = list(writes) + ps
        return list(dict.fromkeys(reads)), list(dict.fromkeys(writes))

    def op(self, eng, fn, reads=(), writes=()):
        reads, writes = self._excl(reads, writes)
        waits = self._deps(eng, reads, writes)
        self.cnt[eng] += 1
        tok = (eng, self.sem[eng], self.cnt[eng])
        self.ops[eng].append((_record_calls(fn), waits, (self.sem[eng], 1)))
        self._record(tok, reads, writes)
        return tok

    def dma(self, q, fn, reads=(), writes=()):
        waits = self._deps(q, reads, writes)
        n = self.dcnt[q]
        self.dcnt[q] += 1
        slot, rnd = n % DMA_POOL, n // DMA_POOL
        sem = self.dsem[q][slot]
        semkey = (q, slot)
        if rnd > 0:
            self._need(q, (semkey, sem, 16 * rnd), waits)
        tok = (semkey, sem, 16 * (rnd + 1))
        self.ops[q].append((_record_calls(fn), waits, (sem, 16)))
        self._record(tok, reads, writes)
        return tok

    def barrier(self):
        toks = list(self.latest.values())
        for e in ENG:
            waits = []
            for t in toks:
                self._need(e, t, waits)
            if waits:
                self.ops[e].append((None, waits, None))
        self.lastw.clear()
        self.readers.clear()

    def final_wait(self, eng, toks):
        waits = []
        for t in toks:
            self._need(eng, t, waits)
        self.ops[eng].append((None, waits, None))

    def emit(self):
        nc = self.nc
        m = {"pe": "tensor", "act": "scalar", "dve": "vector", "pool": "gpsimd", "sp": "sync"}
        with nc.Block() as block:
            for e in ENG:
                lst = self.ops[e]
                if not lst:
                    continue

                def body(engine, lst=lst):
                    for fn, waits, inc in lst:
                        for sem, val in waits:
                            engine.wait_ge(sem, val)
                        if fn is None:
                            continue
                        for name, a, k in fn:
                            ins = getattr(engine, name)(*a, **k)
                        ins.then_inc(inc[0], inc[1])

                getattr(block, m[e])(body)


def _consts():
    i = np.arange(128)
    same = (i[:, None] // 64) == (i[None, :] // 64)
    c = {}
    c["ident"] = np.eye(128, dtype=np.float32)
    c["ones"] = np.ones((128, 128), np.float32)
    c["mle2"] = (same & (i[:, None] <= i[None, :])).astype(np.float32)
    c["mgt2"] = (same & (i[:, None] > i[None, :])).astype(np.float32)
    c["mlt2"] = (same & (i[:, None] < i[None, :])).astype(np.float32)
    c["same2"] = same.astype(np.float32)
    c["half0"] = np.repeat((i < 64).astype(np.float32)[:, None], 128, 1)
    c["half1"] = np.repeat((i >= 64).astype(np.float32)[:, None], 128, 1)
    c["mprev"] = (i[:, None] >= i[None, :]).astype(np.float32)
    c["mcur"] = (i[:, None] <= i[None, :]).astype(np.float32)
    names = list(c.keys())
    arr = np.concatenate([c[n] for n in names], axis=1)
    return names, arr


CONST_NAMES, CONST_ARR = _consts()
NCONST = len(CONST_NAMES)


def build(stop="all", dbg=()):
    nc = bass.Bass("TRN2", target_bir_lowering=False)

    def din(name, shape, dt=F32):
        return nc.dram_tensor(name, list(shape), dt, kind="ExternalInput").ap()

    x_d = din("x", [SEQ, D])
    c_d = din("c", [128, 8])
    wada_d = din("w_ada", [D, 6 * D])
    bada_d = din("b_ada", [1, 6 * D])
    n1w_d = din("n1w", [128, 8])
    n2w_d = din("n2w", [128, 8])
    win_d = din("w_in", [D, D_IN])
    cst_d = din("cst", [128, NCONST * 128])
    convw_d = din("conv_w", [128, 24 * 4])
    alog_d = din("a_log", [1, 8])
    dtb_d = din("dt_bias", [1, 8])
    onw_d = din("onw", [128, 1])
    wa_d = din("w_a", [512, D])
    wb_d = din("w_b", [D, D])
    wo_d = din("w_o", [D, D])
    wr_d = din("w_r", [D, 20])
    br_d = din("b_r", [1, 20])
    wg_d = din("w_eg", [16, D, 256])
    wu_d = din("w_eu", [16, D, 256])
    wd_d = din("w_ed", [16, 256, D])
    qnw_d = din("qnw", [128, 1])
    knw_d = din("knw", [128, 1])
    out_d = nc.dram_tensor("out", [SEQ, D], F32, kind="ExternalOutput").ap()
    dbg_d = {}

    def dbg_out(name, shape):
        if name in dbg:
            dbg_d[name] = nc.dram_tensor("dbg_" + name, list(shape), F32, kind="ExternalOutput").ap()
            return dbg_d[name]
        return None

    out_toks = []
    with contextlib.ExitStack() as st:
        S = Sched(nc, st)

        def sb(stack, name, shape, dt):
            return stack.enter_context(nc.sbuf_tensor(name, list(shape), dt))

        psum = [st.enter_context(nc.psum_tensor(f"ps{i}", [128, 512], F32)) for i in range(8)]
        ps_rr = [0]

        def ps_next():
            i = ps_rr[0]
            ps_rr[0] = (i + 1) % 8
            return i

        cst = sb(st, "cst_sb", [128, NCONST, 128], F32)
        S.dma("sp", lambda e: e.dma_start(out=cst[:].rearrange("p n m -> p (n m)"), in_=cst_d[:, :]), writes=["cst"])

        def C(name):
            return cst[:, CONST_NAMES.index(name), :]

        ident_bf = sb(st, "ident_bf", [128, 128], BF16)
        ones_bf = sb(st, "ones_bf", [128, 128], BF16)
        S.op("dve", lambda e: e.tensor_copy(out=ident_bf[:], in_=C("ident")), reads=["cst"], writes=["ident_bf"])
        S.op("dve", lambda e: e.tensor_copy(out=ones_bf[:], in_=C("ones")), reads=["cst"], writes=["ones_bf"])

        modT = sb(st, "modT", [128, 6, 8], F32)
        A1 = sb(st, "A1", [128, 8], F32)
        A2 = sb(st, "A2", [128, 8], F32)
        g1bc = sb(st, "g1bc", [128, D], F32)
        g2bc = sb(st, "g2bc", [128, D], F32)
        arena = sb(st, "arena", [128, 2 * 8 * SEQ], BF16)
        hT = arena[:, 0:8 * SEQ].rearrange("p (k t) -> p k t", k=8)
        yB = arena[:, 8 * SEQ:16 * SEQ].rearrange("p (k t) -> p k t", k=8)
        x1 = arena[:, :].bitcast(F32).rearrange("p (t f) -> p t f", t=NT)
        ba_tok = sb(st, "ba_tok", [128, NT, 16], F32)
        n2ss = sb(st, "n2ss", [128, NT], F32)

        def norm_stats(tag, src_tile, ss, junk):
            for t in range(NT):
                src, skeys = src_tile(t)
                S.op("act", lambda e, src=src, t=t: e.activation(out=junk[:], in_=src, func=AF.Square, accum_out=ss[:, t:t + 1]),
                     reads=skeys, writes=[tag + "junk", (tag + "ss", t)])

        p01 = contextlib.ExitStack()
        xt = [sb(p01, f"xt{i}", [128, D], F32) for i in range(2)]
        n1junk = sb(p01, "n1junk", [128, D], BF16)
        n1ss = sb(p01, "n1ss", [128, NT], F32)

        def src1(t):
            b = t % 2
            S.dma("sp", lambda e, t=t, b=b: e.dma_start(out=xt[b][:], in_=x_d[t * 128:(t + 1) * 128, :]),
                  writes=[("xt", b)])
            return xt[b][:], [("xt", b)]

        with contextlib.ExitStack() as ph:
            modbc = sb(ph, "modbc", [128, 6 * D], F32)
            csb = sb(ph, "csb", [128, 8], F32)
            cs = sb(ph, "cs", [128, 8], BF16)
            crep = sb(ph, "crep", [128, 8, 128], BF16)
            nw = sb(ph, "nw", [128, 2, 8], F32)
            wab = [sb(ph, f"wab{i}", [128, 8, 512], BF16) for i in range(2)]
            S.dma("sp", lambda e: e.dma_start(out=csb[:], in_=c_d[:, :]), writes=["csb"])
            S.dma("sp", lambda e: e.dma_start(out=nw[:, 0, :], in_=n1w_d[:, :]), writes=["nw0"])
            S.dma("sp", lambda e: e.dma_start(out=nw[:, 1, :], in_=n2w_d[:, :]), writes=["nw1"])
            S.dma("sp", lambda e: e.dma_start(out=modbc[:], in_=bada_d[0:1, :].partition_broadcast(128)),
                  writes=["modbc"])
            S.op("act", lambda e: e.activation(out=cs[:], in_=csb[:], func=AF.Silu), reads=["csb"], writes=["cs"])
            S.op("dve", lambda e: e.tensor_copy(out=crep[:], in_=cs[:].unsqueeze(2).to_broadcast([128, 8, 128])),
                 reads=["cs"], writes=["crep"])
            norm_stats("n1", src1, n1ss, n1junk)
            wada_v = wada_d.rearrange("(k p) n -> p k n", p=128)
            for n in range(12):
                wb = wab[n % 2]
                S.dma("pool", lambda e, wb=wb, n=n: e.dma_start(out=wb[:], in_=wada_v[:, :, n * 512:(n + 1) * 512]),
                      writes=[("wab", n % 2)])
                pi = ps_next()

                def mm(e, wb=wb, pi=pi):
                    for k in range(8):
                        r = e.matmul(psum[pi][:, :], lhsT=crep[:, k, :], rhs=wb[:, k, :], start=(k == 0), stop=(k == 7))
                    return r
                S.op("pe", mm, reads=["crep", ("wab", n % 2)], writes=[("ps", pi)])
                S.op("dve", lambda e, pi=pi, n=n: e.tensor_tensor(
                    out=modbc[:, n * 512:(n + 1) * 512], in0=psum[pi][:, :], in1=modbc[:, n * 512:(n + 1) * 512],
                    op=ALU.add), reads=[("ps", pi), "modbc"], writes=["modbc"])
            for v in range(6):
                for half in range(2):
                    pi = ps_next()

                    def tr(e, v=v, half=half, pi=pi):
                        for j in range(4):
                            c0 = v * D + (half * 4 + j) * 128
                            r = e.transpose(psum[pi][:, j * 128:(j + 1) * 128], modbc[:, c0:c0 + 128], C("ident"))
                        return r
                    S.op("pe", tr, reads=["modbc", "cst"], writes=[("ps", pi)])
                    S.op("dve", lambda e, v=v, half=half, pi=pi: e.tensor_copy(
                        out=modT[:, v, half * 4:half * 4 + 4], in_=psum[pi][:, 0:512:128]),
                        reads=[("ps", pi)], writes=["modT"])
            S.op("dve", lambda e: e.scalar_tensor_tensor(out=A1[:], in0=modT[:, 1, :], scalar=1.0, in1=nw[:, 0, :],
                                                         op0=ALU.add, op1=ALU.mult), reads=["modT", "nw0"], writes=["A1"])
            S.op("dve", lambda e: e.scalar_tensor_tensor(out=A2[:], in0=modT[:, 4, :], scalar=1.0, in1=nw[:, 1, :],
                                                         op0=ALU.add, op1=ALU.mult), reads=["modT", "nw1"], writes=["A2"])
            S.op("pool", lambda e: e.tensor_copy(out=g1bc[:], in_=modbc[:, 2 * D:3 * D]), reads=["modbc"], writes=["g1bc"])
            S.op("pool", lambda e: e.tensor_copy(out=g2bc[:], in_=modbc[:, 5 * D:6 * D]), reads=["modbc"], writes=["g2bc"])
            S.barrier()

        if stop == "p0":
            return _finish(nc, S, sb, st, out_d, out_toks, dbg_d)

        def norm_phase(tag, src_tile, Aw, shift_vec, hT_out, wsm, nsm, sm_out, ss_pre=None):
            with contextlib.ExitStack() as ph:
                if ss_pre is None:
                    junk = sb(ph, tag + "junk", [128, D], BF16)
                    ss = sb(ph, tag + "ss", [128, NT], F32)
                else:
                    ss = ss_pre
                rstd = sb(ph, tag + "rstd", [128, NT], F32)
                xn = [sb(ph, f"{tag}xn{i}", [128, D], F32) for i in range(2)]
                hf = [sb(ph, f"{tag}hf{i}", [128, 8, 128], F32) for i in range(2)]
                tmp = [sb(ph, f"{tag}tmp{i}", [128, 8, 128], F32) for i in range(2)]
                if ss_pre is None:
                    norm_stats(tag, src_tile, ss, junk)
                S.op("act", lambda e: e.activation(out=rstd[:], in_=ss[:], func=AF.Ln, scale=1.0 / D, bias=EPS),
                     reads=[(tag + "ss", t) for t in range(NT)], writes=[tag + "rstd"])
                S.op("act", lambda e: e.activation(out=rstd[:], in_=rstd[:], func=AF.Exp, scale=-0.5),
                     reads=[tag + "rstd"], writes=[tag + "rstd"])

                def stage1(t):
                    src, skeys = src_tile(t)
                    b = t % 2
                    S.op("dve", lambda e, src=src, t=t, b=b: e.tensor_scalar(
                        out=xn[b][:], in0=src, scalar1=rstd[:, t:t + 1], scalar2=None, op0=ALU.mult),
                        reads=list(skeys) + [tag + "rstd"], writes=[(tag + "xn", b)])
                    pis = []
                    for half in range(2):
                        pi = ps_next()
                        pis.append(pi)

                        def tr(e, half=half, pi=pi, b=b):
                            for j in range(4):
                                k = half * 4 + j
                                e.transpose(psum[pi][:, j * 128:(j + 1) * 128], xn[b][:, k * 128:(k + 1) * 128], C("ident"))
                        S.op("pe", tr, reads=[(tag + "xn", b), "cst"], writes=[("ps", pi)])
                    return pis

                def stage2(t, pis):
                    b = t % 2
                    for half in range(2):
                        pi = pis[half]
                        S.op("dve", lambda e, half=half, pi=pi, b=b: e.tensor_tensor(
                            out=tmp[b][:, half * 4:half * 4 + 4, :],
                            in0=psum[pi][:, :].rearrange("p (j m) -> p j m", j=4),
                            in1=Aw[:, half * 4:half * 4 + 4].unsqueeze(2).to_broadcast([128, 4, 128]), op=ALU.mult),
                            reads=[("ps", pi), tag + "A"], writes=[(tag + "tmp", b, half)])
                        S.op("pool", lambda e, half=half, b=b: e.tensor_tensor(
                            out=hf[b][:, half * 4:half * 4 + 4, :], in0=tmp[b][:, half * 4:half * 4 + 4, :],
                            in1=modT[:, shift_vec, half * 4:half * 4 + 4].unsqueeze(2).to_broadcast([128, 4, 128]),
                            op=ALU.add), reads=[(tag + "tmp", b, half), "modT"], writes=[(tag + "hf", b, half)])
                    S.op("act", lambda e, t=t, b=b: e.copy(out=hT_out[:, :, t * 128:(t + 1) * 128], in_=hf[b][:]),
                         reads=[(tag + "hf", b, 0), (tag + "hf", b, 1)], writes=[(tag + "hT", t)])
                    pi = ps_next()

                    def smm(e, pi=pi, b=b):
                        for k in range(8):
                            e.matmul(psum[pi][:, 0:nsm], lhsT=hf[b][:, k, :], rhs=wsm[:, k, :], start=(k == 0), stop=(k == 7))
                    S.op("pe", smm, reads=[(tag + "hf", b, 0), (tag + "hf", b, 1), tag + "wsm"], writes=[("ps", pi)])
                    return pi

                def stage3(t, pi):
                    S.op("dve", lambda e, pi=pi, t=t: e.tensor_copy(out=sm_out[:, t, :], in_=psum[pi][:, 0:nsm]),
                         reads=[("ps", pi)], writes=[(tag + "sm", t)])
                p1 = {0: stage1(0)}
                p2 = {}
                for t in range(NT):
                    if t + 1 < NT:
                        p1[t + 1] = stage1(t + 1)
                    p2[t] = stage2(t, p1[t])
                    if t >= 1:
                        stage3(t - 1, p2[t - 1])
                stage3(NT - 1, p2[NT - 1])
                S.barrier()

        with contextlib.ExitStack() as ph:
            wba = sb(ph, "wba", [128, 8, 16], F32)
            S.dma("sp", lambda e: e.dma_start(out=wba[:], in_=win_d.rearrange("(k p) n -> p k n", p=128)[:, :, OFF_BA:OFF_BA + 16]),
                  writes=["n1wsm"])
            norm_phase("n1", src1, A1, 0, hT, wba, 16, ba_tok, ss_pre=n1ss)
        p01.close()

        d = dbg_out("hT", [128, 8 * SEQ])
        if d is not None:
            with contextlib.ExitStack() as ph:
                hcp = sb(ph, "hcp", [128, 8 * SEQ], F32)
                S.op("dve", lambda e: e.tensor_copy(out=hcp[:], in_=hT[:].rearrange("p k t -> p (k t)")), writes=["hcp"])
                out_toks.append(S.dma("sp", lambda e, d=d: e.dma_start(out=d[:, :], in_=hcp[:]), reads=["hcp"]))
                S.barrier()
        d = dbg_out("ba", [128, NT * 16])
        if d is not None:
            out_toks.append(S.dma("sp", lambda e, d=d: e.dma_start(out=d[:, :], in_=ba_tok[:].rearrange("p t n -> p (t n)")),
                                  reads=[]))

        if stop == "p1":
            return _finish(nc, S, sb, st, out_d, out_toks, dbg_d)

        def dump_fm(name, tens, nk):
            d = dbg_out(name, [128, nk * SEQ])
            if d is None:
                return
            with contextlib.ExitStack() as ph:
                cp = sb(ph, "cp_" + name, [128, nk * SEQ], F32)
                S.op("dve", lambda e: e.tensor_copy(out=cp[:], in_=tens[:].rearrange("p k t -> p (k t)")), writes=["cp"])
                out_toks.append(S.dma("sp", lambda e, d=d: e.dma_start(out=d[:, :], in_=cp[:]), reads=["cp"]))
                S.barrier()

        deltanet_phase(nc, S, sb, st, psum, ps_next, C, ident_bf, ones_bf, hT, ba_tok, yB,
                       win_d, convw_d, alog_d, dtb_d, onw_d, dbg_out, out_toks)
        dump_fm("od", yB, 8)
        zgate_phase(nc, S, sb, psum, ps_next, hT, yB, win_d)
        dump_fm("yb", yB, 8)
        if stop == "dn":
            return _finish(nc, S, sb, st, out_d, out_toks, dbg_d)
        with contextlib.ExitStack() as mx:
            yA = sb(mx, "yA", [128, 4, SEQ], BF16)
            attention_phase(nc, S, sb, psum, ps_next, C, cst, ident_bf, ones_bf, hT, yA, win_d, qnw_d, knw_d)
            dump_fm("ya", yA, 4)
            if stop == "att":
                return _finish(nc, S, sb, st, out_d, out_toks, dbg_d)
            merged = sb(mx, "merged", [128, 8, SEQ], BF16)
            merge_phase(nc, S, sb, psum, ps_next, hT, yA, yB, merged, win_d, wa_d, wb_d)
            dump_fm("merged", merged, 8)
            if stop == "mrg":
                return _finish(nc, S, sb, st, out_d, out_toks, dbg_d)
            wo_phase(nc, S, sb, psum, ps_next, merged, x1, g1bc, x_d, wo_d, n2ss)
            S.barrier()
        d = dbg_out("x1", [128, NT * D])
        if d is not None:
            out_toks.append(S.dma("sp", lambda e, d=d: e.dma_start(out=d[:, :], in_=x1[:].rearrange("p t f -> p (t f)")),
                                  reads=[]))
        if stop == "wo":
            return _finish(nc, S, sb, st, out_d, out_toks, dbg_d)
        with contextlib.ExitStack() as fs:
            h2T = sb(fs, "h2T", [128, 8, SEQ], BF16)
            rl = sb(fs, "rl", [128, NT, 20], F32)
            with contextlib.ExitStack() as ph:
                wr = sb(ph, "wr", [128, 8, 20], F32)
                S.dma("sp", lambda e: e.dma_start(out=wr[:], in_=wr_d.rearrange("(k p) n -> p k n", p=128)), writes=["n2wsm"])
                norm_phase("n2", lambda t: (x1[:, t, :], []), A2, 3, h2T, wr, 20, rl, ss_pre=n2ss)
            moe_phase(nc, S, sb, psum, ps_next, C, ident_bf, h2T, rl, x1, g2bc, br_d, wg_d, wu_d, wd_d, out_d, out_toks)
        S.final_wait("sp", out_toks)
        S.emit()
        return nc, list(dbg_d.keys())


def pk(pi, q0=0, q1=4):
    return [("ps", pi)]


def deltanet_phase(nc, S, sb, st, psum, ps_next, C, ident_bf, ones_bf, hT, ba_tok, yB,
                   win_d, convw_d, alog_d, dtb_d, onw_d, dbg_out, out_toks):
    winv = win_d.rearrange("(k p) n -> p k n", p=128)
    with contextlib.ExitStack() as ph:
        alog = sb(ph, "alog", [128, 8], F32)
        dtb = sb(ph, "dtb", [128, 8], F32)
        onw = sb(ph, "onw_sb", [128, 1], F32)
        convw = sb(ph, "convw", [128, 24, 4], F32)
        S.dma("sp", lambda e: e.dma_start(out=alog[:], in_=alog_d[0:1, :].partition_broadcast(128)), writes=["alog"])
        S.dma("sp", lambda e: e.dma_start(out=dtb[:], in_=dtb_d[0:1, :].partition_broadcast(128)), writes=["dtb"])
        S.dma("sp", lambda e: e.dma_start(out=onw[:], in_=onw_d[:, :]), writes=["onw"])
        S.dma("sp", lambda e: e.dma_start(out=convw[:].rearrange("p c j -> p (c j)"), in_=convw_d[:, :]), writes=["convw"])
        bt = sb(ph, "bt", [128, NT, 8], F32)
        nbt = sb(ph, "nbt", [128, NT, 8], F32)
        gtk = sb(ph, "gtk", [128, NT, 8], F32)
        tmpa = sb(ph, "tmpa", [128, NT, 8], F32)
        negA = sb(ph, "negA", [128, 8], F32)
        gc = sb(ph, "gc", [128, NT, 8], F32)
        negegc = sb(ph, "negegc", [128, NT, 8], F32)
        ksc = sb(ph, "ksc", [128, NT, 8], F32)
        eglb = sb(ph, "eglb", [128, 2, NT, 8], F32)
        S.op("act", lambda e: e.activation(out=bt[:], in_=ba_tok[:, :, 0:8], func=AF.Sigmoid), writes=["bt"])
        S.op("dve", lambda e: e.tensor_scalar(out=nbt[:], in0=bt[:], scalar1=-1.0, scalar2=None, op0=ALU.mult),
             reads=["bt"], writes=["nbt"])
        S.op("dve", lambda e: e.tensor_tensor(out=tmpa[:], in0=ba_tok[:, :, 8:16],
                                              in1=dtb[:].unsqueeze(1).to_broadcast([128, NT, 8]), op=ALU.add),
             reads=["dtb"], writes=["tmpa"])
        S.op("act", lambda e: e.activation(out=tmpa[:], in_=tmpa[:], func=AF.Exp), reads=["tmpa"], writes=["tmpa"])
        S.op("act", lambda e: e.activation(out=tmpa[:], in_=tmpa[:], func=AF.Ln, bias=1.0), reads=["tmpa"], writes=["tmpa"])
        S.op("act", lambda e: e.activation(out=negA[:], in_=alog[:], func=AF.Exp), reads=["alog"], writes=["negA"])
        S.op("dve", lambda e: e.tensor_scalar(out=negA[:], in0=negA[:], scalar1=-1.0, scalar2=None, op0=ALU.mult),
             reads=["negA"], writes=["negA"])
        S.op("dve", lambda e: e.tensor_tensor(out=gtk[:], in0=tmpa[:], in1=negA[:].unsqueeze(1).to_broadcast([128, NT, 8]),
                                              op=ALU.mult), reads=["tmpa", "negA"], writes=["gtk"])
        g2 = gtk[:].rearrange("p t h -> p (t h)")
        pi = ps_next()

        def mmg(e, pi=pi):
            e.matmul(psum[pi][:, 0:128], lhsT=C("mle2"), rhs=g2, start=True, stop=True)
            e.matmul(psum[pi][:, 128:256], lhsT=C("same2"), rhs=g2, start=True, stop=True)
            e.matmul(psum[pi][:, 256:384], lhsT=C("half0"), rhs=g2, start=True, stop=True)
            return e.matmul(psum[pi][:, 384:512], lhsT=C("half1"), rhs=g2, start=True, stop=True)
        S.op("pe", mmg, reads=["gtk", "cst"], writes=pk(pi))
        S.op("dve", lambda e, pi=pi: e.tensor_copy(out=gc[:].rearrange("p t h -> p (t h)"), in_=psum[pi][:, 0:128]),
             reads=pk(pi), writes=["gc"])
        S.op("act", lambda e, pi=pi: e.activation(out=negegc[:].rearrange("p t h -> p (t h)"), in_=psum[pi][:, 0:128],
                                                  func=AF.Exp), reads=pk(pi), writes=["negegc"])
        S.op("dve", lambda e: e.tensor_scalar(out=negegc[:], in0=negegc[:], scalar1=-1.0, scalar2=None, op0=ALU.mult),
             reads=["negegc"], writes=["negegc"])
        S.op("dve", lambda e, pi=pi: e.tensor_tensor(out=ksc[:].rearrange("p t h -> p (t h)"), in0=psum[pi][:, 128:256],
                                                     in1=gc[:].rearrange("p t h -> p (t h)"), op=ALU.subtract),
             reads=pk(pi) + ["gc"], writes=["ksc"])
        S.op("act", lambda e: e.activation(out=ksc[:], in_=ksc[:], func=AF.Exp), reads=["ksc"], writes=["ksc"])
        S.op("act", lambda e, pi=pi: e.activation(out=eglb[:].rearrange("p a t h -> p (a t h)"), in_=psum[pi][:, 256:512],
                                                  func=AF.Exp), reads=pk(pi), writes=["eglb"])

        W = sb(ph, "dnW", [128, 8, 1536], BF16)
        diag = sb(ph, "diag", [128, 12, 4, 128], BF16)
        raw = sb(ph, "raw", [128, 12, 515], BF16)
        cs = sb(ph, "cs_dn", [128, 12, 512], BF16)
        sq = [sb(ph, f"sq_dn{i}", [128, 512], BF16) for i in range(3)]
        lnv = [sb(ph, f"lnv_dn{i}", [128, 512], F32) for i in range(3)]
        qdT = sb(ph, "qdT", [128, 4, 512], BF16)
        ktok = sb(ph, "ktok", [128, 4, 4, 128], BF16)
        vtok = sb(ph, "vtok", [128, 4, 4, 128], BF16)
        Gm2 = [sb(ph, f"Gm2_{i}", [128, 128], F32) for i in range(4)]
        EE = [sb(ph, f"EE{i}", [128, 256], F32) for i in range(4)]
        E2m = [sb(ph, f"E2m{i}", [128, 128], F32) for i in range(4)]
        E2s = [sb(ph, f"E2s{i}", [128, 128], F32) for i in range(4)]
        LB = [sb(ph, f"LB{i}", [128, 384], BF16) for i in range(8)]
        TT = sb(ph, "TT", [128, 4, 4, 128], BF16)
        intraT = sb(ph, "intraT", [128, 4, 4, 128], BF16)
        Sst = sb(ph, "Sst", [128, 4, 128], F32)
        Sbf = sb(ph, "Sbf", [128, 4, 128], BF16)
        Zt = sb(ph, "Zt", [128, 4, 128], BF16)
        vnew = sb(ph, "vnew", [128, 4, 128], BF16)
        obuf = sb(ph, "obuf", [128, 4, 512], F32)
        osq = sb(ph, "osq", [128, 512], BF16)
        lb_rr = [0]

        for hg in range(2):
            for ty, off in enumerate((OFF_QB, OFF_KB, OFF_VB)):
                S.dma("pool", lambda e, ty=ty, off=off: e.dma_start(
                    out=W[:, :, ty * 512:(ty + 1) * 512], in_=winv[:, :, off + hg * 512: off + (hg + 1) * 512]),
                    writes=[("dnW", ty)])
            for ci in range(12):
                gch = (ci // 4) * 8 + hg * 4 + (ci % 4)
                for j in range(4):
                    S.op("dve", lambda e, ci=ci, j=j, gch=gch: e.tensor_scalar(
                        out=diag[:, ci, j, :], in0=ident_bf[:], scalar1=convw[:, gch, j:j + 1], scalar2=None,
                        op0=ALU.mult), reads=["convw", "ident_bf"], writes=[("diag", ci)])
            S.op("pool", lambda e: e.memset(raw[:, :, 0:3], 0.0), writes=[("raw", ci) for ci in range(12)])
            S.op("dve", lambda e: e.memset(Sst[:], 0.0), writes=[("S", h) for h in range(4)])
            S.op("dve", lambda e: e.memset(Sbf[:], 0.0), writes=[("Sbf", h) for h in range(4)])

            for sc in range(4):
                tok0 = sc * 512
                for ci in range(12):
                    ty, hl = ci // 4, ci % 4
                    pi = ps_next()

                    def proj(e, ci=ci, ty=ty, hl=hl, pi=pi):
                        for k in range(8):
                            e.matmul(psum[pi][:, :], lhsT=W[:, k, ty * 512 + hl * 128: ty * 512 + (hl + 1) * 128],
                                     rhs=hT[:, k, tok0:tok0 + 512], start=(k == 0), stop=(k == 7))
                    S.op("pe", proj, reads=[("dnW", ty)], writes=pk(pi))
                    S.op("dve", lambda e, ci=ci, pi=pi: e.tensor_copy(out=raw[:, ci, 3:515], in_=psum[pi][:, :]),
                         reads=pk(pi), writes=[("raw", ci)])
                for ci in range(12):
                    pj = ps_next()

                    def conv(e, ci=ci, pj=pj):
                        for j in range(4):
                            e.matmul(psum[pj][:, :], lhsT=diag[:, ci, j, :], rhs=raw[:, ci, j:j + 512],
                                     start=(j == 0), stop=(j == 3))
                    S.op("pe", conv, reads=[("raw", ci), ("diag", ci)], writes=pk(pj))
                    S.op("act", lambda e, ci=ci, pj=pj: e.activation(out=cs[:, ci, :], in_=psum[pj][:, :], func=AF.Silu),
                         reads=pk(pj), writes=[("cs", ci)])
                    S.op("pool", lambda e, ci=ci: e.tensor_copy(out=raw[:, ci, 0:3], in_=raw[:, ci, 512:515]),
                         reads=[("raw", ci)], writes=[("raw", ci)])
                def tok_major(hl, src_ci, scaled):
                    pi = ps_next()
                    pbf = psum[pi][:, :].bitcast(BF16)

                    def trk(e, src_ci=src_ci, pbf=pbf):
                        for tt in range(4):
                            e.transpose(pbf[:, tt * 128:(tt + 1) * 128], cs[:, src_ci, tt * 128:(tt + 1) * 128], ident_bf[:])
                    S.op("pe", trk, reads=[("cs", src_ci), "ident_bf"], writes=pk(pi))
                    if scaled:
                        h = hg * 4 + hl
                        for tt in range(4):
                            T = sc * 4 + tt
                            S.op("dve", lambda e, hl=hl, tt=tt, T=T, h=h, pbf=pbf: e.tensor_scalar(
                                out=ktok[:, hl, tt, :], in0=pbf[:, tt * 128:(tt + 1) * 128], scalar1=ksc[:, T, h:h + 1],
                                scalar2=None, op0=ALU.mult), reads=pk(pi) + ["ksc"], writes=[("ktok", hl)])
                    else:
                        S.op("act", lambda e, hl=hl, pbf=pbf: e.copy(out=vtok[:, hl, :, :].rearrange("p a b -> p (a b)"),
                                                                    in_=pbf[:, 0:512]), reads=pk(pi), writes=[("vtok", hl)])
                for hl in range(4):
                    tok_major(hl, 8 + hl, False)
                order = [4, 5, 6, 7, 0, 1, 2, 3]
                pqs = {}

                def n_stage1(i):
                    ci = order[i]
                    lb = i % 3
                    S.op("dve", lambda e, ci=ci, lb=lb: e.tensor_tensor(out=sq[lb][:], in0=cs[:, ci, :], in1=cs[:, ci, :], op=ALU.mult),
                         reads=[("cs", ci)], writes=[("sq", lb)])
                    pq = ps_next()
                    pqs[i] = pq
                    S.op("pe", lambda e, pq=pq, lb=lb: e.matmul(psum[pq][:, :], lhsT=ones_bf[:], rhs=sq[lb][:], start=True, stop=True),
                         reads=[("sq", lb), "ones_bf"], writes=pk(pq))

                def n_stage2(i):
                    ci = order[i]
                    ty = ci // 4
                    lb = i % 3
                    pq = pqs[i]
                    S.op("act", lambda e, pq=pq, lb=lb: e.activation(out=lnv[lb][:], in_=psum[pq][:, :], func=AF.Ln, bias=EPS),
                         reads=pk(pq), writes=[("lnv", lb)])
                    S.op("act", lambda e, lb=lb: e.activation(out=lnv[lb][:], in_=lnv[lb][:], func=AF.Exp, scale=-0.5),
                         reads=[("lnv", lb)], writes=[("lnv", lb)])
                    scl = 128.0 ** -0.5 if ty == 0 else 1.0
                    S.op("dve", lambda e, ci=ci, lb=lb, scl=scl: e.scalar_tensor_tensor(
                        out=cs[:, ci, :], in0=cs[:, ci, :], scalar=scl, in1=lnv[lb][:], op0=ALU.mult, op1=ALU.mult),
                        reads=[("cs", ci), ("lnv", lb)], writes=[("cs", ci)])
                    if ty == 1:
                        tok_major(ci - 4, ci, True)
                n_stage1(0)
                n_stage1(1)
                for i in range(8):
                    if i + 2 < 8:
                        n_stage1(i + 2)
                    n_stage2(i)
                b_rr = [0]

                def ps_b():
                    i = 4 + b_rr[0] % 4
                    b_rr[0] += 1
                    return i

                def gen_B(tt):
                    T = sc * 4 + tt
                    tk = slice(tt * 128, (tt + 1) * 128)
                    cur = {}
                    pxs = {}
                    for hl in range(4):
                        h = hg * 4 + hl
                        S.op("dve", lambda e, bb=hl, h=h: e.tensor_scalar(
                            out=Gm2[bb][:], in0=C("mle2"), scalar1=gtk[:, T, h:h + 1], scalar2=None, op0=ALU.mult),
                            reads=["gtk", "cst"], writes=[("Gm2", hl)])
                    for hl in range(4):
                        px = ps_b()
                        pxs[hl] = px

                        h = hg * 4 + hl
                        S.op("pe", lambda e, bb=hl, px=px: e.matmul(psum[px][:, 0:128], lhsT=C("ones"), rhs=Gm2[bb][:], start=True, stop=True),
                             reads=[("Gm2", hl), "cst"], writes=pk(px))
                        S.op("dve", lambda e, bb=hl, px=px, h=h: e.tensor_scalar(
                            out=EE[bb][:, 0:128], in0=psum[px][:, 0:128], scalar1=gc[:, T, h:h + 1], scalar2=0.0,
                            op0=ALU.subtract, op1=ALU.min), reads=pk(px) + ["gc"], writes=[("EE", hl, 0)])
                        S.op("act", lambda e, bb=hl, px=px: e.activation(out=EE[bb][:, 128:256], in_=psum[px][:, 0:128], func=AF.Exp),
                             reads=pk(px), writes=[("EE", hl, 1)])
                        S.op("act", lambda e, bb=hl: e.activation(out=EE[bb][:, 0:128], in_=EE[bb][:, 0:128], func=AF.Exp),
                             reads=[("EE", hl, 0)], writes=[("EE", hl, 0)])
                    yield
                    for hl in range(4):
                        S.op("pool", lambda e, bb=hl: e.tensor_tensor(out=E2m[bb][:], in0=EE[bb][:, 0:128], in1=C("mle2"), op=ALU.mult),
                             reads=[("EE", hl, 0), "cst"], writes=[("E2m", hl)])
                        S.op("dve", lambda e, bb=hl: e.tensor_tensor(out=E2s[bb][:], in0=EE[bb][:, 0:128], in1=C("mlt2"), op=ALU.mult),
                             reads=[("EE", hl, 0), "cst"], writes=[("E2s", hl)])
                    yield
                    for hl in range(4):
                        h = hg * 4 + hl
                        bb = hl
                        py = ps_b()

                        def mmk(e, hl=hl, py=py):
                            e.matmul(psum[py][:, 0:128], lhsT=cs[:, 4 + hl, tk], rhs=cs[:, 4 + hl, tk], start=True, stop=True)
                            e.matmul(psum[py][:, 128:256], lhsT=cs[:, 4 + hl, tk], rhs=cs[:, hl, tk], start=True, stop=True)
                        S.op("pe", mmk, reads=[("cs", 4 + hl), ("cs", hl)], writes=pk(py))
                        l0 = 2 * hl
                        cur[hl] = l0
                        S.op("dve", lambda e, l0=l0, py=py, bb=bb, h=h: e.scalar_tensor_tensor(
                            out=LB[l0][:, 0:128], in0=psum[py][:, 0:128], scalar=nbt[:, T, h:h + 1], in1=E2s[bb][:],
                            op0=ALU.mult, op1=ALU.mult), reads=pk(py) + [("E2s", bb), "nbt"], writes=[("LB", l0, 0)])
                        S.op("dve", lambda e, hl=hl, py=py, bb=bb: e.tensor_tensor(
                            out=intraT[:, hl, tt, :], in0=psum[py][:, 128:256], in1=E2m[bb][:], op=ALU.mult),
                            reads=pk(py) + [("E2m", bb)], writes=[("intraT", hl, tt)])
                        S.op("pool", lambda e, hl=hl, bb=bb: e.tensor_tensor(
                            out=qdT[:, hl, tk], in0=cs[:, hl, tk], in1=EE[bb][:, 128:256], op=ALU.mult),
                            reads=[("cs", hl), ("EE", bb, 1)], writes=[("qdT", hl, tt)])
                        if hl % 2 == 1:
                            yield
                    for hl in range(4):
                        l0 = cur[hl]
                        pz = ps_b()
                        pzb = psum[pz][:, :].bitcast(BF16)
                        S.op("pe", lambda e, l0=l0, pzb=pzb: e.transpose(pzb[:, 0:128], LB[l0][:, 0:128], ident_bf[:]),
                             reads=[("LB", l0, 0), "ident_bf"], writes=pk(pz))
                        S.op("act", lambda e, l0=l0, pzb=pzb: e.copy(out=LB[l0][:, 256:384], in_=pzb[:, 0:128]),
                             reads=pk(pz), writes=[("LB", l0, 2)])
                    yield
                    for lev in range(6):
                        last = lev == 5
                        for hl in range(4):
                            c_ = cur[hl]
                            pl = ps_b()
                            if not last:
                                nxt = 2 * hl + (1 - (c_ % 2))

                                def mml(e, c_=c_, pl=pl, lev=lev):
                                    if lev == 0:
                                        e.matmul(psum[pl][:, 0:128], lhsT=LB[c_][:, 256:384], rhs=LB[c_][:, 0:128], start=True, stop=False)
                                        e.matmul(psum[pl][:, 128:256], lhsT=LB[c_][:, 256:384], rhs=ident_bf[:], start=False, stop=False)
                                        e.matmul(psum[pl][:, 128:256], lhsT=ident_bf[:], rhs=ident_bf[:], start=False, stop=False)
                                    else:
                                        e.matmul(psum[pl][:, 0:256], lhsT=LB[c_][:, 256:384], rhs=LB[c_][:, 0:256], start=True, stop=False)
                                        e.matmul(psum[pl][:, 128:256], lhsT=ident_bf[:], rhs=LB[c_][:, 128:256], start=False, stop=False)
                                    e.matmul(psum[pl][:, 256:384], lhsT=LB[c_][:, 0:128], rhs=LB[c_][:, 256:384], start=False, stop=True)
                                rdl = [("LB", c_, 0), ("LB", c_, 2), "ident_bf"] + ([("LB", c_, 1)] if lev > 0 else [])
                                S.op("pe", mml, reads=rdl, writes=pk(pl))
                                wr = [("LB", nxt, 0), ("LB", nxt, 1), ("LB", nxt, 2)]
                                if (lev + hl) % 2 == 0:
                                    S.op("act", lambda e, nxt=nxt, pl=pl: e.copy(out=LB[nxt][:, :], in_=psum[pl][:, 0:384]),
                                         reads=pk(pl), writes=wr)
                                else:
                                    S.op("dve", lambda e, nxt=nxt, pl=pl: e.tensor_copy(out=LB[nxt][:, :], in_=psum[pl][:, 0:384]),
                                         reads=pk(pl), writes=wr)
                                cur[hl] = nxt
                            else:
                                def mml2(e, c_=c_, pl=pl):
                                    e.matmul(psum[pl][:, 0:128], lhsT=LB[c_][:, 256:384], rhs=LB[c_][:, 128:256], start=True, stop=False)
                                    e.matmul(psum[pl][:, 0:128], lhsT=ident_bf[:], rhs=LB[c_][:, 128:256], start=False, stop=True)
                                S.op("pe", mml2, reads=[("LB", c_, 1), ("LB", c_, 2), "ident_bf"], writes=pk(pl))
                                if hl % 2 == 0:
                                    S.op("act", lambda e, hl=hl, pl=pl: e.copy(out=TT[:, hl, tt, :], in_=psum[pl][:, 0:128]),
                                         reads=pk(pl), writes=[("TT", hl, tt)])
                                else:
                                    S.op("dve", lambda e, hl=hl, pl=pl: e.tensor_copy(out=TT[:, hl, tt, :], in_=psum[pl][:, 0:128]),
                                         reads=pk(pl), writes=[("TT", hl, tt)])
                            if hl % 2 == 1:
                                yield

                def gen_C(tt):
                    T = sc * 4 + tt
                    for half in range(2):
                        b0 = 64 * half
                        rows = slice(b0, b0 + 64)
                        ctok = slice(tt * 128 + b0, tt * 128 + b0 + 64)
                        pA = [0, 1]
                        pB = [2, 3]
                        for pr in range(2):
                            def mm1(e, pr=pr):
                                for hl in (2 * pr, 2 * pr + 1):
                                    c0 = (hl % 2) * 128
                                    e.matmul(psum[pA[pr]][rows, c0:c0 + 128], lhsT=cs[:, 4 + hl, ctok], rhs=Sbf[:, hl, :],
                                             start=True, stop=True)
                            S.op("pe", mm1, reads=[("cs", 4 + 2 * pr), ("cs", 5 + 2 * pr), ("Sbf", 2 * pr), ("Sbf", 2 * pr + 1)],
                                 writes=pk(pA[pr]))
                        yield
                        for pr in range(2):
                            for hl in (2 * pr, 2 * pr + 1):
                                h = hg * 4 + hl
                                c0 = (hl % 2) * 128
                                S.op("dve", lambda e, hl=hl, c0=c0, pr=pr, h=h: e.scalar_tensor_tensor(
                                    out=Zt[rows, hl, :], in0=psum[pA[pr]][rows, c0:c0 + 128], scalar=negegc[rows, T, h:h + 1],
                                    in1=vtok[rows, hl, tt, :], op0=ALU.mult, op1=ALU.add),
                                    reads=pk(pA[pr]) + ["negegc", ("vtok", hl)], writes=[("Zt", hl, half)])
                        yield
                        for pr in range(2):
                            def mm2(e, pr=pr):
                                for hl in (2 * pr, 2 * pr + 1):
                                    c0 = (hl % 2) * 128
                                    e.matmul(psum[pB[pr]][rows, c0:c0 + 128], lhsT=TT[rows, hl, tt, rows], rhs=Zt[rows, hl, :],
                                             start=True, stop=True)
                            S.op("pe", mm2, reads=[("TT", 2 * pr, tt), ("TT", 2 * pr + 1, tt), ("Zt", 2 * pr, half), ("Zt", 2 * pr + 1, half)],
                                 writes=pk(pB[pr]))
                        yield
                        for pr in range(2):
                            for hl in (2 * pr, 2 * pr + 1):
                                h = hg * 4 + hl
                                c0 = (hl % 2) * 128
                                S.op("act", lambda e, hl=hl, c0=c0, pr=pr, h=h: e.activation(
                                    out=vnew[rows, hl, :], in_=psum[pB[pr]][rows, c0:c0 + 128], func=AF.Copy, scale=bt[rows, T, h:h + 1]),
                                    reads=pk(pB[pr]) + ["bt"], writes=[("vnew", hl, half)])
                        yield
                        pO = pA
                        pS = pB
                        for pr in range(2):
                            def mm3(e, pr=pr):
                                for hl in (2 * pr, 2 * pr + 1):
                                    c0 = (hl % 2) * 128
                                    e.matmul(psum[pO[pr]][:, c0:c0 + 64], lhsT=Sbf[:, hl, :], rhs=qdT[:, hl, ctok], start=True, stop=False)
                                    e.matmul(psum[pO[pr]][:, c0:c0 + 64], lhsT=vnew[rows, hl, :], rhs=intraT[rows, hl, tt, rows],
                                             start=False, stop=True)
                                for hl in (2 * pr, 2 * pr + 1):
                                    c0 = (hl % 2) * 128
                                    e.matmul(psum[pS[pr]][:, c0:c0 + 128], lhsT=ktok[rows, hl, tt, :], rhs=vnew[rows, hl, :],
                                             start=True, stop=True)
                            rd = []
                            for hl in (2 * pr, 2 * pr + 1):
                                rd += [("Sbf", hl), ("qdT", hl, tt), ("vnew", hl, half), ("intraT", hl, tt), ("ktok", hl)]
                            S.op("pe", mm3, reads=rd, writes=pk(pO[pr]) + pk(pS[pr]))
                        yield
                        for pr in range(2):
                            for hl in (2 * pr, 2 * pr + 1):
                                h = hg * 4 + hl
                                c0 = (hl % 2) * 128
                                S.op("dve", lambda e, hl=hl, c0=c0, pr=pr, h=h: e.scalar_tensor_tensor(
                                    out=Sst[:, hl, :], in0=Sst[:, hl, :], scalar=eglb[:, half, T, h:h + 1], in1=psum[pS[pr]][:, c0:c0 + 128],
                                    op0=ALU.mult, op1=ALU.add), reads=[("S", hl), "eglb"] + pk(pS[pr]), writes=[("S", hl)])
                                S.op("act", lambda e, hl=hl: e.copy(out=Sbf[:, hl, :], in_=Sst[:, hl, :]),
                                     reads=[("S", hl)], writes=[("Sbf", hl)])
                        yield
                        for pr in range(2):
                            for hl in (2 * pr, 2 * pr + 1):
                                c0 = (hl % 2) * 128
                                S.op("dve", lambda e, hl=hl, c0=c0, pr=pr: e.tensor_copy(
                                    out=obuf[:, hl, ctok], in_=psum[pO[pr]][:, c0:c0 + 64]),
                                    reads=pk(pO[pr]), writes=[("obuf", hl)])
                        yield

                def run_interleaved(gens):
                    gens = [g for g in gens if g is not None]
                    while gens:
                        for g in list(gens):
                            try:
                                next(g)
                            except StopIteration:
                                gens.remove(g)
                run_interleaved([gen_B(0)])
                for tt in range(4):
                    run_interleaved([gen_C(tt), gen_B(tt + 1) if tt < 3 else None])
                for hl in range(4):
                    h = hg * 4 + hl
                    lb = hl % 2
                    S.op("act", lambda e, hl=hl: e.activation(out=osq[:], in_=obuf[:, hl, :], func=AF.Square),
                         reads=[("obuf", hl)], writes=["osq"])
                    pq = ps_next()
                    S.op("pe", lambda e, pq=pq: e.matmul(psum[pq][:, :], lhsT=ones_bf[:], rhs=osq[:], start=True, stop=True),
                         reads=["osq", "ones_bf"], writes=pk(pq))
                    S.op("act", lambda e, pq=pq, lb=lb: e.activation(out=lnv[lb][:], in_=psum[pq][:, :], func=AF.Ln,
                                                                    scale=1.0 / 128, bias=EPS),
                         reads=pk(pq), writes=[("lnv", lb)])
                    S.op("act", lambda e, lb=lb: e.activation(out=lnv[lb][:], in_=lnv[lb][:], func=AF.Exp, scale=-0.5),
                         reads=[("lnv", lb)], writes=[("lnv", lb)])
                    S.op("dve", lambda e, hl=hl, h=h, lb=lb: e.scalar_tensor_tensor(
                        out=yB[:, h, tok0:tok0 + 512], in0=obuf[:, hl, :], scalar=onw[:, 0:1], in1=lnv[lb][:],
                        op0=ALU.mult, op1=ALU.mult), reads=[("obuf", hl), ("lnv", lb), "onw"], writes=[("yB", h)])
        S.barrier()


def zgate_phase(nc, S, sb, psum, ps_next, hT, yB, win_d):
    winv = win_d.rearrange("(k p) n -> p k n", p=128)
    with contextlib.ExitStack() as ph:
        Wz = sb(ph, "Wz", [128, 8, 1024], BF16)
        szb = [sb(ph, f"szb{i}", [128, 512], F32) for i in range(2)]
        for i in range(2):
            S.dma("pool", lambda e, i=i: e.dma_start(out=Wz[:, :, i * 512:(i + 1) * 512],
                                                     in_=winv[:, :, OFF_ZB + i * 512:OFF_ZB + (i + 1) * 512]), writes=[("Wz", i)])
        for h in range(8):
            for T4 in range(4):
                pi = ps_next()
                b = (h * 4 + T4) % 2

                def mz(e, h=h, T4=T4, pi=pi):
                    for k in range(8):
                        e.matmul(psum[pi][:, :], lhsT=Wz[:, k, h * 128:(h + 1) * 128], rhs=hT[:, k, T4 * 512:(T4 + 1) * 512],
                                 start=(k == 0), stop=(k == 7))
                S.op("pe", mz, reads=[("Wz", h // 4)], writes=pk(pi))
                S.op("act", lambda e, pi=pi, b=b: e.activation(out=szb[b][:], in_=psum[pi][:, :], func=AF.Silu),
                     reads=pk(pi), writes=[("szb", b)])
                S.op("dve", lambda e, h=h, T4=T4, b=b: e.tensor_tensor(
                    out=yB[:, h, T4 * 512:(T4 + 1) * 512], in0=yB[:, h, T4 * 512:(T4 + 1) * 512], in1=szb[b][:], op=ALU.mult),
                    reads=[("szb", b), ("yB", h)], writes=[("yB", h)])
        S.barrier()


def attention_phase(nc, S, sb, psum, ps_next, C, cst, ident_bf, ones_bf, hT, yA, win_d, qnw_d, knw_d):
    winv = win_d.rearrange("(k p) n -> p k n", p=128)
    imask = CONST_NAMES.index("mprev")
    mask2 = cst[:, imask:imask + 2, :].rearrange("p a b -> p (a b)")
    with contextlib.ExitStack() as ph:
        qkw = sb(ph, "qkw", [128, 2], F32)
        S.dma("sp", lambda e: e.dma_start(out=qkw[:, 0:1], in_=qnw_d[:, :]), writes=["qkw0"])
        S.dma("sp", lambda e: e.dma_start(out=qkw[:, 1:2], in_=knw_d[:, :]), writes=["qkw1"])
        Wb = [[sb(ph, f"attW{i}_{t}", [128, 8, 256], BF16) for t in range(3)] for i in range(2)]
        qP = sb(ph, "qP", [128, 2, SEQ], BF16)
        kP = sb(ph, "kP", [128, 2, SEQ], BF16)
        vP = sb(ph, "vP", [128, 16, 256], BF16)
        accn = sb(ph, "accn", [128, 2, SEQ], F32)
        accd = sb(ph, "accd", [128, 2, SEQ], F32)
        sq = [sb(ph, f"sq_at{i}", [128, 512], BF16) for i in range(2)]
        maskneg = sb(ph, "maskneg", [128, 256], BF16)
        S.op("dve", lambda e: e.tensor_scalar(out=maskneg[:], in0=mask2, scalar1=30000.0, scalar2=-30000.0,
                                              op0=ALU.mult, op1=ALU.add), reads=["cst"], writes=["maskneg"])
        lnv = [sb(ph, f"lnv_at{i}", [128, 512], F32) for i in range(2)]
        Eb = [sb(ph, f"Eb{i}", [128, 256], BF16) for i in range(4)]
        it = 0
        eb_rr = 0
        for hp in range(2):
            for g, d in enumerate((1, 4, 16)):
                L = SEQ // d
                nb = L // 128
                wb = Wb[it % 2]
                it += 1
                for t, off in enumerate((OFF_QA, OFF_KA, OFF_VA)):
                    c0 = off + g * 512 + hp * 256
                    S.dma("pool", lambda e, t=t, c0=c0, wb=wb: e.dma_start(out=wb[t][:], in_=winv[:, :, c0:c0 + 256]),
                          writes=[("attW", id(wb), t)])
                tiles = [(t, dst, j, T4) for t, dst in ((0, qP), (1, kP)) for j in range(2) for T4 in range(4)]

                def issue_proj(idx):
                    t, dst, j, T4 = tiles[idx]
                    pi = ps_next()

                    def pq(e, t=t, j=j, T4=T4, pi=pi, wb=wb):
                        for k in range(8):
                            e.matmul(psum[pi][:, :], lhsT=wb[t][:, k, j * 128:(j + 1) * 128],
                                     rhs=hT[:, k, T4 * 512:(T4 + 1) * 512], start=(k == 0), stop=(k == 7))
                    S.op("pe", pq, reads=[("attW", id(wb), t)], writes=pk(pi))
                    return pi
                pnext = issue_proj(0)
                for idx in range(len(tiles)):
                    t, dst, j, T4 = tiles[idx]
                    pi = pnext
                    if idx + 1 < len(tiles):
                        pnext = issue_proj(idx + 1)
                    lb = idx % 2
                    S.op("act", lambda e, pi=pi, lb=lb: e.activation(out=sq[lb][:], in_=psum[pi][:, :], func=AF.Square),
                         reads=pk(pi), writes=[("sq", lb)])
                    p2 = ps_next()
                    S.op("pe", lambda e, p2=p2, lb=lb: e.matmul(psum[p2][:, :], lhsT=ones_bf[:], rhs=sq[lb][:], start=True, stop=True),
                         reads=[("sq", lb)], writes=pk(p2))
                    S.op("act", lambda e, p2=p2, lb=lb: e.activation(out=lnv[lb][:], in_=psum[p2][:, :], func=AF.Ln,
                                                                    scale=1.0 / 128, bias=EPS),
                         reads=pk(p2), writes=[("lnv", lb)])
                    S.op("act", lambda e, lb=lb: e.activation(out=lnv[lb][:], in_=lnv[lb][:], func=AF.Exp, scale=-0.5),
                         reads=[("lnv", lb)], writes=[("lnv", lb)])
                    m0 = T4 * 512 // d
                    ov = dst[:, j, :].rearrange("p (r m) -> p r m", r=d)[:, :, m0:m0 + 512 // d]
                    S.op("dve", lambda e, pi=pi, lb=lb, ov=ov, t=t: e.scalar_tensor_tensor(
                        out=ov, in0=psum[pi][:, :].rearrange("p (m r) -> p r m", r=d), scalar=qkw[:, t:t + 1],
                        in1=lnv[lb][:].rearrange("p (m r) -> p r m", r=d), op0=ALU.mult, op1=ALU.mult),
                        reads=pk(pi) + [("lnv", lb), "qkw0", "qkw1"], writes=[("qkP", t, j)])
                for b2 in range(8):
                    pi = ps_next()

                    def pv(e, b2=b2, pi=pi, wb=wb):
                        for bb in range(2):
                            blk = b2 * 2 + bb
                            r, n = blk // nb, blk % nb
                            s0 = r + d * 128 * n
                            for k in range(8):
                                e.matmul(psum[pi][:, bb * 256:(bb + 1) * 256], lhsT=hT[:, k, s0:s0 + 127 * d + 1:d],
                                         rhs=wb[2][:, k, :], start=(k == 0), stop=(k == 7))
                    S.op("pe", pv, reads=[("attW", id(wb), 2)], writes=pk(pi))
                    S.op("act", lambda e, b2=b2, pi=pi: e.copy(out=vP[:, b2 * 2:b2 * 2 + 2, :].rearrange("p a b -> p (a b)"),
                                                               in_=psum[pi][:, :]), reads=pk(pi), writes=[("vP", b2)])
                blocks = [(j, blk) for j in range(2) for blk in range(16)]

                def issue_sc(idx):
                    nonlocal eb_rr
                    j, blk = blocks[idx]
                    n = blk % nb
                    kbs = ([(blk - 1, 0)] if n > 0 else []) + [(blk, 1)]
                    c_lo = kbs[0][1] * 128
                    psc = ps_next()

                    def sc(e, j=j, blk=blk, kbs=kbs, psc=psc, c_lo=c_lo):
                        for i, (kb, part) in enumerate(kbs):
                            e.matmul(psum[psc][:, part * 128:(part + 1) * 128], lhsT=kP[:, j, kb * 128:(kb + 1) * 128],
                                     rhs=qP[:, j, blk * 128:(blk + 1) * 128], start=(i == 0), stop=False)
                        e.matmul(psum[psc][:, c_lo:256], lhsT=ident_bf[:], rhs=maskneg[:, c_lo:256], start=False, stop=True)
                    S.op("pe", sc, reads=[("qkP", 0, j), ("qkP", 1, j), "maskneg"], writes=pk(psc))
                    eb = eb_rr % 4
                    eb_rr += 1
                    return psc, eb, kbs, c_lo
                ahead = [issue_sc(0), issue_sc(1)]
                pn = pd = None
                for idx in range(len(blocks)):
                    j, blk = blocks[idx]
                    b4, qb = blk // 4, blk % 4
                    psc, eb, kbs, c_lo = ahead.pop(0)
                    if idx + 2 < len(blocks):
                        ahead.append(issue_sc(idx + 2))
                    S.op("act", lambda e, psc=psc, eb=eb, c_lo=c_lo: e.activation(
                        out=Eb[eb][:, c_lo:256], in_=psum[psc][:, c_lo:256], func=AF.Exp, scale=128.0 ** -0.5),
                        reads=pk(psc), writes=[("Eb", eb)])
                    if qb == 0:
                        pn, pd = ps_next(), ps_next()

                    def pvm(e, j=j, qb=qb, kbs=kbs, eb=eb, pn=pn, pd=pd):
                        for i, (kb, part) in enumerate(kbs):
                            e.matmul(psum[pn][:, qb * 128:(qb + 1) * 128], lhsT=vP[:, kb, j * 128:(j + 1) * 128],
                                     rhs=Eb[eb][:, part * 128:(part + 1) * 128], start=(i == 0), stop=(i == len(kbs) - 1))
                        for i, (kb, part) in enumerate(kbs):
                            e.matmul(psum[pd][:, qb * 128:(qb + 1) * 128], lhsT=ones_bf[:],
                                     rhs=Eb[eb][:, part * 128:(part + 1) * 128], start=(i == 0), stop=(i == len(kbs) - 1))
                    S.op("pe", pvm, reads=[("Eb", eb)] + [("vP", kb // 2) for kb, _ in kbs], writes=pk(pn) + pk(pd))
                    if qb == 3:
                        for acc, pb in ((accn, pn), (accd, pd)):
                            if d == 1:
                                av = acc[:, j, b4 * 512:(b4 + 1) * 512]
                                bv = psum[pb][:, :]
                            elif d == 4:
                                av = acc[:, j, b4:SEQ:4]
                                bv = psum[pb][:, :]
                            else:
                                av = acc[:, j, :].rearrange("p (i s) -> p s i", s=16)[:, b4 * 4:b4 * 4 + 4, :]
                                bv = psum[pb][:, :].rearrange("p (r i) -> p r i", r=4)
                            if g == 0:
                                S.op("dve", lambda e, av=av, bv=bv: e.tensor_copy(out=av, in_=bv),
                                     reads=pk(pb), writes=[("acc", id(acc), j)])
                            else:
                                S.op("dve", lambda e, av=av, bv=bv: e.tensor_tensor(out=av, in0=av, in1=bv, op=ALU.add),
                                     reads=pk(pb) + [("acc", id(acc), j)], writes=[("acc", id(acc), j)])
            for j in range(2):
                S.op("act", lambda e, j=j: e.activation(out=accd[:, j, :], in_=accd[:, j, :], func=AF.Ln),
                     reads=[("acc", id(accd), j)], writes=[("acc", id(accd), j)])
                S.op("act", lambda e, j=j: e.activation(out=accd[:, j, :], in_=accd[:, j, :], func=AF.Exp, scale=-1.0),
                     reads=[("acc", id(accd), j)], writes=[("acc", id(accd), j)])
                S.op("dve", lambda e, j=j, hp=hp: e.tensor_tensor(out=yA[:, 2 * hp + j, :], in0=accn[:, j, :], in1=accd[:, j, :],
                                                                  op=ALU.mult),
                     reads=[("acc", id(accd), j), ("acc", id(accn), j)], writes=[("yA", 2 * hp + j)])
        S.barrier()


def merge_phase(nc, S, sb, psum, ps_next, hT, yA, yB, merged, win_d, wa_d, wb_d):
    winv = win_d.rearrange("(k p) n -> p k n", p=128)
    with contextlib.ExitStack() as ph:
        Wga = sb(ph, "Wga", [128, 8, D], BF16)
        Wgb = sb(ph, "Wgb", [128, 8, D], BF16)
        WA = sb(ph, "WA", [128, 4, D], BF16)
        WB = sb(ph, "WB", [128, 8, D], BF16)
        for i in range(2):
            cs_ = slice(i * 512, (i + 1) * 512)
            S.dma("pool", lambda e, i=i, cs_=cs_: e.dma_start(out=WA[:, :, cs_], in_=wa_d.rearrange("(k p) n -> p k n", p=128)[:, :, cs_]),
                  writes=[("WA", i)])
            S.dma("pool", lambda e, i=i, cs_=cs_: e.dma_start(out=Wga[:, :, cs_], in_=winv[:, :, OFF_GA + i * 512:OFF_GA + (i + 1) * 512]),
                  writes=[("Wga", i)])
            S.dma("pool", lambda e, i=i, cs_=cs_: e.dma_start(out=WB[:, :, cs_], in_=wb_d.rearrange("(k p) n -> p k n", p=128)[:, :, cs_]),
                  writes=[("WB", i)])
            S.dma("pool", lambda e, i=i, cs_=cs_: e.dma_start(out=Wgb[:, :, cs_], in_=winv[:, :, OFF_GB + i * 512:OFF_GB + (i + 1) * 512]),
                  writes=[("Wgb", i)])
        sg = [sb(ph, f"sg{i}", [128, 512], F32) for i in range(2)]
        m1 = [sb(ph, f"m1_{i}", [128, 512], F32) for i in range(2)]
        for c in range(8):
            cc = slice(c * 128, (c + 1) * 128)
            wi = c // 4
            for T4 in range(4):
                tk = slice(T4 * 512, (T4 + 1) * 512)
                for br, (Wp, nk, src, Wg, key) in enumerate(((WA, 4, yA, Wga, "A"), (WB, 8, yB, Wgb, "B"))):
                    pp, pg = ps_next(), ps_next()

                    def mmp(e, Wp=Wp, nk=nk, src=src, pp=pp, cc=cc, tk=tk):
                        for k in range(nk):
                            e.matmul(psum[pp][:, :], lhsT=Wp[:, k, cc], rhs=src[:, k, tk], start=(k == 0), stop=(k == nk - 1))
                    S.op("pe", mmp, reads=[("W" + key, wi)], writes=pk(pp))

                    def mmg(e, Wg=Wg, pg=pg, cc=cc, tk=tk):
                        for k in range(8):
                            e.matmul(psum[pg][:, :], lhsT=Wg[:, k, cc], rhs=hT[:, k, tk], start=(k == 0), stop=(k == 7))
                    S.op("pe", mmg, reads=[("Wg" + key.lower(), wi)], writes=pk(pg))
                    S.op("act", lambda e, pg=pg, br=br: e.activation(out=sg[br][:], in_=psum[pg][:, :], func=AF.Sigmoid),
                         reads=pk(pg), writes=[("sg", br)])
                    S.op("dve", lambda e, pp=pp, br=br: e.tensor_tensor(out=m1[br][:], in0=psum[pp][:, :], in1=sg[br][:], op=ALU.mult),
                         reads=pk(pp) + [("sg", br)], writes=[("m1", br)])
                S.op("pool", lambda e, c=c, tk=tk: e.tensor_tensor(out=merged[:, c, tk], in0=m1[0][:], in1=m1[1][:], op=ALU.add),
                     reads=[("m1", 0), ("m1", 1)], writes=[("merged", c)])
        S.barrier()


def wo_phase(nc, S, sb, psum, ps_next, merged, x1, g1bc, x_d, wo_d, n2ss):
    with contextlib.ExitStack() as ph:
        Wo = sb(ph, "Wo", [128, 8, D], BF16)
        for i in range(2):
            S.dma("pool", lambda e, i=i: e.dma_start(out=Wo[:, :, i * 512:(i + 1) * 512],
                                                     in_=wo_d.rearrange("(k p) n -> p k n", p=128)[:, :, i * 512:(i + 1) * 512]),
                  writes=[("Wo", i)])
        xt = [sb(ph, f"xt_wo{i}", [128, D], F32) for i in range(2)]
        tmp = [sb(ph, f"tmp_wo{i}", [128, 512], F32) for i in range(2)]
        junk = sb(ph, "wojunk", [128, D], BF16)
        for t in range(NT):
            b = t % 2
            S.dma("sp", lambda e, t=t, b=b: e.dma_start(out=xt[b][:], in_=x_d[t * 128:(t + 1) * 128, :]), writes=[("xt", b)])
            for ch in range(2):
                cc = slice(ch * 512, (ch + 1) * 512)
                pi = ps_next()

                def mm(e, t=t, cc=cc, pi=pi):
                    for k in range(8):
                        e.matmul(psum[pi][:, :], lhsT=merged[:, k, t * 128:(t + 1) * 128], rhs=Wo[:, k, cc], start=(k == 0), stop=(k == 7))
                S.op("pe", mm, reads=[("Wo", ch)], writes=pk(pi))
                S.op("dve", lambda e, pi=pi, ch=ch, cc=cc: e.tensor_tensor(out=tmp[ch][:], in0=psum[pi][:, :], in1=g1bc[:, cc], op=ALU.mult),
                     reads=pk(pi), writes=[("tmp", ch)])
                S.op("pool", lambda e, t=t, b=b, ch=ch, cc=cc: e.tensor_tensor(out=x1[:, t, cc], in0=tmp[ch][:], in1=xt[b][:, cc], op=ALU.add),
                     reads=[("tmp", ch), ("xt", b)], writes=[("x1", t, ch)])
            S.op("act", lambda e, t=t: e.activation(out=junk[:], in_=x1[:, t, :], func=AF.Square, accum_out=n2ss[:, t:t + 1]),
                 reads=[("x1", t, 0), ("x1", t, 1)], writes=["wojunk", ("n2ss", t)])
        S.barrier()


def moe_phase(nc, S, sb, psum, ps_next, C, ident_bf, h2T, rl, x1, g2bc, br_d, wg_d, wu_d, wd_d, out_d, out_toks):
    BIG = 1.0e30
    with contextlib.ExitStack() as ph:
        cwT = sb(ph, "cwT", [16, 2, SEQ], BF16)
        sel = sb(ph, "sel", [16, 16, 128], BF16)
        S.op("dve", lambda e: e.tensor_copy(out=sel[:], in_=ident_bf[0:16, 0:16].unsqueeze(2).to_broadcast([16, 16, 128])),
             writes=["sel"])
        Wgu = [sb(ph, f"Wgu{i}", [128, 2, 8, 256], BF16) for i in range(2)]

        def load_gu(ex2):
            wb2 = Wgu[ex2 % 2]
            S.dma("pool", lambda e: e.dma_start(out=wb2[:, 0, :, :], in_=wg_d[ex2].rearrange("(k p) f -> p k f", p=128)),
                  writes=[("Wgu", ex2 % 2, 0)])
            S.dma("pool", lambda e: e.dma_start(out=wb2[:, 1, :, :], in_=wu_d[ex2].rearrange("(k p) f -> p k f", p=128)),
                  writes=[("Wgu", ex2 % 2, 1)])
        load_gu(0)
        with contextlib.ExitStack() as rs:
            brb = sb(rs, "brb", [128, 20], F32)
            S.dma("sp", lambda e: e.dma_start(out=brb[:], in_=br_d[0:1, :].partition_broadcast(128)), writes=["brb"])
            gmax = sb(rs, "gmax", [128, NT], F32)
            ohg = sb(rs, "ohg", [128, NT, 4], F32)
            eg = sb(rs, "eg", [128, NT, 4], F32)
            ptop = sb(rs, "ptop", [128, NT], F32)
            pen = sb(rs, "pen", [128, NT, 4], F32)
            elm = sb(rs, "elm", [128, NT, 16], F32)
            m1 = sb(rs, "m1r", [128, NT], F32)
            m2 = sb(rs, "m2r", [128, NT], F32)
            oh1 = sb(rs, "oh1", [128, NT, 16], F32)
            oh2 = sb(rs, "oh2", [128, NT, 16], F32)
            w1 = sb(rs, "w1", [128, NT], F32)
            w2 = sb(rs, "w2", [128, NT], F32)
            cw = sb(rs, "cw", [128, NT, 16], F32)
            cwTf = sb(rs, "cwTf", [16, SEQ], F32)
            cwTr = sb(rs, "cwTr", [16, SEQ], F32)
            K = "rt"
            def bc(ap, n):
                return ap.unsqueeze(2).to_broadcast([128, NT, n])
            S.op("dve", lambda e: e.tensor_tensor(out=rl[:], in0=rl[:], in1=brb[:].unsqueeze(1).to_broadcast([128, NT, 20]), op=ALU.add),
                 reads=["brb"], writes=[K])
            gl = rl[:, :, 0:4]
            el = rl[:, :, 4:20]
            S.op("dve", lambda e: e.tensor_reduce(out=gmax[:], in_=gl, axis=mybir.AxisListType.X, op=ALU.max), reads=[K], writes=[K])
            S.op("dve", lambda e: e.tensor_tensor(out=ohg[:], in0=gl, in1=bc(gmax[:], 4), op=ALU.is_equal), reads=[K], writes=[K])
            S.op("dve", lambda e: e.tensor_tensor(out=eg[:], in0=gl, in1=bc(gmax[:], 4), op=ALU.subtract), reads=[K], writes=[K])
            S.op("act", lambda e: e.activation(out=eg[:], in_=eg[:], func=AF.Exp), reads=[K], writes=[K])
            S.op("dve", lambda e: e.tensor_reduce(out=ptop[:], in_=eg[:], axis=mybir.AxisListType.X, op=ALU.add), reads=[K], writes=[K])
            S.op("dve", lambda e: e.reciprocal(out=ptop[:], in_=ptop[:]), reads=[K], writes=[K])
            S.op("dve", lambda e: e.tensor_scalar(out=pen[:], in0=ohg[:], scalar1=BIG, scalar2=-BIG, op0=ALU.mult, op1=ALU.add),
                 reads=[K], writes=[K])
            S.op("dve", lambda e: e.tensor_tensor(out=elm[:].rearrange("p t (g e) -> p t g e", g=4),
                                                  in0=el.rearrange("p t (g e) -> p t g e", g=4),
                                                  in1=pen[:].unsqueeze(3).to_broadcast([128, NT, 4, 4]), op=ALU.add),
                 reads=[K], writes=[K])
            S.op("dve", lambda e: e.tensor_reduce(out=m1[:], in_=elm[:], axis=mybir.AxisListType.X, op=ALU.max), reads=[K], writes=[K])
            S.op("dve", lambda e: e.tensor_tensor(out=oh1[:], in0=elm[:], in1=bc(m1[:], 16), op=ALU.is_equal), reads=[K], writes=[K])
            S.op("dve", lambda e: e.scalar_tensor_tensor(out=elm[:].rearrange("p t e -> p (t e)"), in0=oh1[:].rearrange("p t e -> p (t e)"),
                                                         scalar=-BIG, in1=elm[:].rearrange("p t e -> p (t e)"),
                                                         op0=ALU.mult, op1=ALU.add), reads=[K], writes=[K])
            S.op("dve", lambda e: e.tensor_reduce(out=m2[:], in_=elm[:], axis=mybir.AxisListType.X, op=ALU.max), reads=[K], writes=[K])
            S.op("dve", lambda e: e.tensor_tensor(out=oh2[:], in0=elm[:], in1=bc(m2[:], 16), op=ALU.is_equal), reads=[K], writes=[K])
            S.op("dve", lambda e: e.tensor_tensor(out=w1[:], in0=m1[:], in1=m2[:], op=ALU.subtract), reads=[K], writes=[K])
            S.op("act", lambda e: e.activation(out=w1[:], in_=w1[:], func=AF.Sigmoid), reads=[K], writes=[K])
            S.op("dve", lambda e: e.tensor_scalar(out=w2[:], in0=w1[:], scalar1=-1.0, scalar2=1.0, op0=ALU.mult, op1=ALU.add),
                 reads=[K], writes=[K])
            S.op("dve", lambda e: e.tensor_tensor(out=w1[:], in0=w1[:], in1=ptop[:], op=ALU.mult), reads=[K], writes=[K])
            S.op("dve", lambda e: e.tensor_tensor(out=w2[:], in0=w2[:], in1=ptop[:], op=ALU.mult), reads=[K], writes=[K])
            S.op("dve", lambda e: e.tensor_tensor(out=oh1[:], in0=oh1[:], in1=bc(w1[:], 16), op=ALU.mult), reads=[K], writes=[K])
            S.op("dve", lambda e: e.tensor_tensor(out=oh2[:], in0=oh2[:], in1=bc(w2[:], 16), op=ALU.mult), reads=[K], writes=[K])
            S.op("dve", lambda e: e.tensor_tensor(out=cw[:], in0=oh1[:], in1=oh2[:], op=ALU.add), reads=[K], writes=[K])
            for q in range(4):
                pi = ps_next()

                def trc(e, q=q, pi=pi):
                    for j in range(4):
                        t = q * 4 + j
                        e.transpose(psum[pi][0:16, j * 128:(j + 1) * 128], cw[:, t, :], C("ident"))
                S.op("pe", trc, reads=[K, "cst"], writes=pk(pi))
                S.op("dve", lambda e, q=q, pi=pi: e.tensor_copy(out=cwTf[:, q * 512:(q + 1) * 512], in_=psum[pi][0:16, :]),
                     reads=pk(pi), writes=[("cwTf", q)])
            rq = [("cwTf", q) for q in range(4)]
            S.op("dve", lambda e: e.tensor_copy(out=cwT[:, 0, :], in_=cwTf[:]), reads=rq, writes=["cwT0"])
            S.op("dve", lambda e: e.tensor_tensor(out=cwTr[:], in0=cwTf[:], in1=cwT[:, 0, :], op=ALU.subtract),
                 reads=rq + ["cwT0"], writes=["cwTr"])
            S.op("dve", lambda e: e.tensor_copy(out=cwT[:, 1, :], in_=cwTr[:]), reads=["cwTr"], writes=["cwT1"])
            S.barrier()

        hid = sb(ph, "hid", [128, 4, 2, SEQ], BF16)
        Wd2 = [sb(ph, "Wd0", [128, 4, 2, D], BF16)] * 2
        sgb = [sb(ph, f"sgm{i}", [128, 512], F32) for i in range(2)]
        t1 = [sb(ph, f"t1m{i}", [128, 512], F32) for i in range(2)]
        tmo = [sb(ph, f"tmo{i}", [128, 512], F32) for i in range(2)]
        it = 0
        for gi in range(4):
            Wd = Wd2[gi % 2]
            for el_ in range(4):
                ex = gi * 4 + el_
                wb = Wgu[ex % 2]
                if ex + 1 < 16:
                    load_gu(ex + 1)
                S.dma("pool", lambda e, ex=ex, el_=el_, Wd=Wd: e.dma_start(out=Wd[:, el_, :, :], in_=wd_d[ex].rearrange("(c p) n -> p c n", p=128)),
                      writes=[("Wd", id(Wd), el_)])
                for T4 in range(4):
                    tk = slice(T4 * 512, (T4 + 1) * 512)
                    pc = ps_next()

                    def mmc(e, ex=ex, tk=tk, pc=pc):
                        e.matmul(psum[pc][:, :], lhsT=sel[:, ex, :], rhs=cwT[:, 0, tk], start=True, stop=False)
                        e.matmul(psum[pc][:, :], lhsT=sel[:, ex, :], rhs=cwT[:, 1, tk], start=False, stop=True)
                    S.op("pe", mmc, reads=["sel", "cwT0", "cwT1"], writes=pk(pc))
                    for ffc in range(2):
                        fc = slice(ffc * 128, (ffc + 1) * 128)
                        pg, pu = ps_next(), ps_next()

                        def mmgu(e, wb=wb, fc=fc, tk=tk, pg=pg, pu=pu):
                            for k in range(8):
                                e.matmul(psum[pg][:, :], lhsT=wb[:, 0, k, fc], rhs=h2T[:, k, tk], start=(k == 0), stop=(k == 7))
                            for k in range(8):
                                e.matmul(psum[pu][:, :], lhsT=wb[:, 1, k, fc], rhs=h2T[:, k, tk], start=(k == 0), stop=(k == 7))
                        S.op("pe", mmgu, reads=[("Wgu", ex % 2, 0), ("Wgu", ex % 2, 1)], writes=pk(pg) + pk(pu))
                        b = it % 2
                        it += 1
                        S.op("act", lambda e, pg=pg, b=b: e.activation(out=sgb[b][:], in_=psum[pg][:, :], func=AF.Silu),
                             reads=pk(pg), writes=[("sgm", b)])
                        S.op("dve", lambda e, pu=pu, b=b: e.tensor_tensor(out=t1[b][:], in0=psum[pu][:, :], in1=sgb[b][:], op=ALU.mult),
                             reads=pk(pu) + [("sgm", b)], writes=[("t1m", b)])
                        S.op("dve", lambda e, pc=pc, b=b, el_=el_, ffc=ffc, tk=tk: e.tensor_tensor(
                            out=hid[:, el_, ffc, tk], in0=psum[pc][:, :], in1=t1[b][:], op=ALU.mult),
                            reads=pk(pc) + [("t1m", b)], writes=[("hid", el_)])
            for t in range(NT):
                for ch in range(2):
                    cc = slice(ch * 512, (ch + 1) * 512)
                    po = ps_next()

                    def mmd(e, t=t, cc=cc, po=po, Wd=Wd):
                        i = 0
                        for e2 in range(4):
                            for ffc in range(2):
                                e.matmul(psum[po][:, :], lhsT=hid[:, e2, ffc, t * 128:(t + 1) * 128], rhs=Wd[:, e2, ffc, cc],
                                         start=(i == 0), stop=(i == 7))
                                i += 1
                    S.op("pe", mmd, reads=[("hid", e2) for e2 in range(4)] + [("Wd", id(Wd), e2) for e2 in range(4)], writes=pk(po))
                    S.op("dve", lambda e, po=po, ch=ch, cc=cc: e.tensor_tensor(out=tmo[ch][:], in0=psum[po][:, :], in1=g2bc[:, cc], op=ALU.mult),
                         reads=pk(po), writes=[("tmo", ch)])
                    S.op("pool", lambda e, t=t, ch=ch, cc=cc: e.tensor_tensor(out=x1[:, t, cc], in0=x1[:, t, cc], in1=tmo[ch][:], op=ALU.add),
                         reads=[("tmo", ch), ("x1", t, ch)], writes=[("x1", t, ch)])
                if gi == 3:
                    out_toks.append(S.dma("sp", lambda e, t=t: e.dma_start(out=out_d[t * 128:(t + 1) * 128, :], in_=x1[:, t, :]),
                                          reads=[("x1", t, 0), ("x1", t, 1)]))
        S.barrier()


def _finish(nc, S, sb, st, out_d, out_toks, dbg_d, zero_out=True):
    if zero_out:
        with contextlib.ExitStack() as ph:
            z = sb(ph, "zz", [128, D], F32)
            S.op("dve", lambda e: e.memset(z[:], 0.0), writes=["zz"])
            for t in range(NT):
                out_toks.append(S.dma("sp", lambda e, t=t: e.dma_start(out=out_d[t * 128:(t + 1) * 128, :], in_=z[:]),
                                      reads=["zz"]))
            S.barrier()
    S.final_wait("sp", out_toks)
    S.emit()
    return nc, list(dbg_d.keys())


def _prep_inputs(inputs):
    f = lambda a: np.ascontiguousarray(np.asarray(a, dtype=np.float32))
    shared = {
        "w_ada": f(inputs["w_ada"][0]),
        "b_ada": f(inputs["b_ada"][0][None, :]),
        "n1w": f(inputs["norm1_w"][0].reshape(8, 128).T),
        "n2w": f(inputs["norm2_w"][0].reshape(8, 128).T),
        "w_in": f(inputs["w_in"][0]),
        "cst": f(CONST_ARR),
        "conv_w": f(inputs["conv_w"][0].reshape(4, 24, 128).transpose(2, 1, 0).reshape(128, 96)),
        "a_log": f(inputs["b_A_log"][0][None, :]),
        "dt_bias": f(inputs["b_dt_bias"][0][None, :]),
        "onw": f(inputs["b_out_norm_w"][0][:, None]),
        "qnw": f(inputs["a_q_norm_w"][0][:, None]),
        "w_a": f(inputs["w_branch_a"][0]),
        "w_b": f(inputs["w_branch_b"][0]),
        "w_o": f(inputs["w_o"][0]),
        "w_r": f(np.concatenate([inputs["w_router_group"][0], inputs["w_router_expert"][0]], axis=1)),
        "b_r": f(np.concatenate([inputs["b_router_group"][0].reshape(-1), inputs["b_router_expert"][0].reshape(-1)])[None, :]),
        "w_eg": f(inputs["w_exp_gate"][0]),
        "w_eu": f(inputs["w_exp_up"][0]),
        "w_ed": f(inputs["w_exp_down"][0]),
        "knw": f(inputs["a_k_norm_w"][0][:, None]),
    }
    maps = []
    for b in range(8):
        m = dict(shared)
        m["x"] = f(inputs["x"][b])
        m["c"] = f(inputs["c"][b].reshape(8, 128).T)
        maps.append(m)
    return maps


def kernel(**inputs):
    nc, _ = build()
    maps = _prep_inputs(inputs)
    res = run_bass_kernel_spmd(nc, maps, core_ids=list(range(8)))
    return np.stack([np.asarray(r["out"], dtype=np.float32) for r in res.results], axis=0)
```
